# Optimizing a Trainium2 kernel written in Bass

```python
import math
import jax, jax.numpy as jnp
from jax import lax
import numpy as np


D_MODEL = 1024
BATCH = 8
SEQ = 4096
DEPTH = 4

N_MIXERS = 4
D_FF = 2816
MEM_LEN = 256
X_HEADS = 4
X_HEAD_DIM = 128
X_WIDTH = X_HEADS * X_HEAD_DIM
POOL_WINDOWS = (2, 4, 8, 16)
POOL_GROUPS = len(POOL_WINDOWS)
POOL_GROUP_DIM = D_MODEL // POOL_GROUPS
DIFF_HEAD_DIM = 64
DIFF_HEADS = D_MODEL // (2 * DIFF_HEAD_DIM)
DIFF_QK = DIFF_HEADS * 2 * DIFF_HEAD_DIM
DIFF_V = DIFF_HEADS * 2 * DIFF_HEAD_DIM
Q_BLOCK = 128
MLSTM_HEADS = 4
MLSTM_HEAD_DIM = D_MODEL // MLSTM_HEADS
MLSTM_CONV = 4
MLSTM_CHUNK = 64
GLA_HEADS = 4
GLA_KEY_DIM = D_MODEL // 2 // GLA_HEADS
GLA_VALUE_DIM = D_MODEL // GLA_HEADS
GLA_GATE_RANK = 16
GLA_TAU = 16.0
GLA_CHUNK = 64
NORM_EPS = 1e-6
SUBLN_EPS = 1e-5
POOL_IN = D_MODEL + X_WIDTH
DIFF_IN = 2 * DIFF_QK + DIFF_V + X_WIDTH
MLSTM_IN = 4 * D_MODEL + 2 * MLSTM_HEADS + X_WIDTH
GLA_IN = 2 * GLA_HEADS * GLA_KEY_DIM + 2 * D_MODEL + GLA_GATE_RANK + X_WIDTH
MIX_OUT = D_MODEL + X_WIDTH

kernel_name = 'hybrid_interleaved_pool_diff_mlstm_gla'


def rmsnorm(x, g, eps=NORM_EPS):
    x32 = x.astype(jnp.float32)
    y = x32 * lax.rsqrt(jnp.mean(x32 * x32, axis=-1, keepdims=True) + eps)
    return (y * g.astype(jnp.float32)).astype(x.dtype)


def swiglu(h, w_up, w_down):
    gate, up = jnp.split(h @ w_up, 2, axis=-1)
    return (jax.nn.silu(gate) * up) @ w_down


def causal_conv(x, w):
    K, C = w.shape
    return lax.conv_general_dilated(x, w[:, None, :].astype(x.dtype), (1,), [(K - 1, 0)],
                                    dimension_numbers=('NWC', 'WIO', 'NWC'), feature_group_count=C)


def alibi_slopes(n):
    return jnp.asarray([2.0 ** (-8.0 * (h + 1) / n) for h in range(n)], dtype=jnp.float32)


def memory_attention(xq, mem_k, mem_v):
    B, S, _ = xq.shape
    q = xq.reshape(B, S, X_HEADS, X_HEAD_DIM)
    s = jnp.einsum('bshd,bmhd->bhsm', q, mem_k).astype(jnp.float32) * (X_HEAD_DIM ** -0.5)
    p = jax.nn.softmax(s, axis=-1)
    o = jnp.einsum('bhsm,bmhd->bshd', p.astype(mem_v.dtype), mem_v)
    return o.reshape(B, S, X_WIDTH)


def _chunks(a, L):
    B, H, S = a.shape[:3]
    return jnp.moveaxis(a.reshape(B, H, S // L, L, *a.shape[3:]), 2, 0)


def _unchunk(a):
    NC, B, H, L = a.shape[:4]
    return jnp.moveaxis(a, 0, 2).reshape(B, H, NC * L, *a.shape[4:])


def multiscale_pool(u, w_group, scale):
    B, S, _ = u.shape
    u32 = u.astype(jnp.float32).reshape(B, S, POOL_GROUPS, POOL_GROUP_DIM)
    cs = jnp.cumsum(u32, axis=1)
    outs = []
    for g, w in enumerate(POOL_WINDOWS):
        c = cs[:, :, g]
        prev = jnp.pad(c[:, :S - w], ((0, 0), (w, 0), (0, 0)))
        count = jnp.minimum(jnp.arange(1, S + 1), w).astype(jnp.float32)
        outs.append((c - prev) / count[None, :, None] - u32[:, :, g])
    pooled = jnp.stack(outs, axis=2).astype(u.dtype)
    mixed = jnp.einsum('bsgc,gcd->bsgd', pooled, w_group).reshape(B, S, D_MODEL)
    return mixed * scale


def pool_layer(h, w_in, w_group, scale, w_out, mem_k, mem_v):
    proj = h @ w_in
    u, xq = proj[..., :D_MODEL], proj[..., D_MODEL:]
    mix = multiscale_pool(u, w_group, scale)
    xo = memory_attention(xq, mem_k, mem_v)
    return jnp.concatenate([mix, xo], axis=-1) @ w_out


def diff_attention(q, k, v, lam, slopes):
    S = q.shape[3]
    scale = DIFF_HEAD_DIM ** -0.5
    outs = []
    for start in range(0, S, Q_BLOCK):
        end = start + Q_BLOCK
        dist = (jnp.arange(start, end)[:, None] - jnp.arange(end)[None, :]).astype(jnp.float32)
        bias = jnp.where(dist >= 0, -slopes[:, None, None] * dist, -jnp.inf)
        s = jnp.einsum('bhcqd,bhckd->bhcqk', q[:, :, :, start:end], k[:, :, :, :end]).astype(jnp.float32) * scale
        p = jax.nn.softmax(s + bias[None, :, None], axis=-1)
        w = p[:, :, 0] - lam * p[:, :, 1]
        outs.append(jnp.einsum('bhqk,bhkd->bhqd', w.astype(v.dtype), v[:, :, :end]))
    return jnp.concatenate(outs, axis=2)


def diff_layer(h, w_in, lam_p, norm_g, w_out, mem_k, mem_v, slopes, layer_idx):
    B, S, _ = h.shape
    H, Dh = DIFF_HEADS, DIFF_HEAD_DIM
    proj = h @ w_in
    q = proj[..., :DIFF_QK].reshape(B, S, H, 2, Dh).transpose(0, 2, 3, 1, 4)
    k = proj[..., DIFF_QK:2 * DIFF_QK].reshape(B, S, H, 2, Dh).transpose(0, 2, 3, 1, 4)
    v = proj[..., 2 * DIFF_QK:2 * DIFF_QK + DIFF_V].reshape(B, S, H, 2 * Dh).transpose(0, 2, 1, 3)
    xq = proj[..., 2 * DIFF_QK + DIFF_V:]
    lam_init = 0.8 - 0.6 * math.exp(-0.3 * layer_idx)
    lp = lam_p.astype(jnp.float32)
    lam = jnp.exp(jnp.sum(lp[0] * lp[1])) - jnp.exp(jnp.sum(lp[2] * lp[3])) + lam_init
    o = diff_attention(q, k, v, lam, slopes)
    o = rmsnorm(o, norm_g, eps=SUBLN_EPS) * (1.0 - lam_init)
    o = o.transpose(0, 2, 1, 3).reshape(B, S, H * 2 * Dh)
    xo = memory_attention(xq, mem_k, mem_v)
    return jnp.concatenate([o, xo], axis=-1) @ w_out


def mlstm_chunkwise(q, k, v, i_pre, f_pre):
    B, H, S, Dk = q.shape
    Dv = v.shape[-1]
    L = MLSTM_CHUNK
    q = q * (Dk ** -0.5)
    lf = jax.nn.log_sigmoid(f_pre)
    causal = jnp.tril(jnp.ones((L, L), dtype=bool))

    def step(carry, inp):
        C, n, m = carry
        qc, kc, vc, ic, lfc = inp
        b = jnp.cumsum(lfc, axis=-1)
        dmat = jnp.where(causal, b[..., :, None] - b[..., None, :] + ic[..., None, :], -jnp.inf)
        inter = b + m[..., None]
        m_t = jnp.maximum(inter, jnp.max(dmat, axis=-1))
        dec = jnp.exp(inter - m_t)
        sqk = jnp.einsum('bhtd,bhsd->bhts', qc, kc) * jnp.exp(dmat - m_t[..., None])
        num = dec[..., None] * jnp.einsum('bhtd,bhde->bhte', qc, C) + jnp.einsum('bhts,bhse->bhte', sqk, vc)
        den = dec * jnp.einsum('bhtd,bhd->bht', qc, n) + jnp.sum(sqk, axis=-1)
        hc = num / jnp.maximum(jnp.abs(den), jnp.exp(-m_t))[..., None]
        bL = b[..., -1]
        gs = bL[..., None] - b + ic
        m_new = jnp.maximum(bL + m, jnp.max(gs, axis=-1))
        ws = jnp.exp(gs - m_new[..., None])
        carry_dec = jnp.exp(bL + m - m_new)
        C_new = carry_dec[..., None, None] * C + jnp.einsum('bhs,bhsd,bhse->bhde', ws, kc, vc)
        n_new = carry_dec[..., None] * n + jnp.einsum('bhs,bhsd->bhd', ws, kc)
        return (C_new, n_new, m_new), hc

    init = (jnp.zeros((B, H, Dk, Dv), jnp.float32), jnp.zeros((B, H, Dk), jnp.float32),
            jnp.zeros((B, H), jnp.float32))
    xs = (_chunks(q, L), _chunks(k, L), _chunks(v, L), _chunks(i_pre, L), _chunks(lf, L))
    _, hs = lax.scan(step, init, xs)
    return _unchunk(hs)


def mlstm_layer(h, w_in, conv_w, gate_b, norm_g, w_out, mem_k, mem_v):
    B, S, _ = h.shape
    H, Dh, D = MLSTM_HEADS, MLSTM_HEAD_DIM, D_MODEL
    proj = h @ w_in
    qk = jax.nn.silu(causal_conv(proj[..., :2 * D], conv_w))
    q, k = qk[..., :D], qk[..., D:]
    v = proj[..., 2 * D:3 * D]
    o = proj[..., 3 * D:4 * D]
    gates = proj[..., 4 * D:4 * D + 2 * H].astype(jnp.float32).reshape(B, S, 2, H) + gate_b.astype(jnp.float32)
    xq = proj[..., 4 * D + 2 * H:]

    def heads(a):
        return a.reshape(B, S, H, Dh).transpose(0, 2, 1, 3).astype(jnp.float32)

    hh = mlstm_chunkwise(heads(q), heads(k), heads(v),
                         gates[:, :, 0].transpose(0, 2, 1), gates[:, :, 1].transpose(0, 2, 1))
    hh = rmsnorm(hh.transpose(0, 2, 1, 3).astype(h.dtype), norm_g.reshape(H, Dh)).reshape(B, S, D)
    hh = hh * jax.nn.sigmoid(o)
    xo = memory_attention(xq, mem_k, mem_v)
    return jnp.concatenate([hh, xo], axis=-1) @ w_out


def gla_chunkwise(q, k, v, log_a):
    B, H, S, Dk = q.shape
    Dv = v.shape[-1]
    L = GLA_CHUNK
    q = q * (Dk ** -0.5)
    causal = jnp.tril(jnp.ones((L, L), dtype=bool))

    def step(state, inp):
        qc, kc, vc, ac = inp
        b = jnp.cumsum(ac, axis=2)
        inter = jnp.einsum('bhtd,bhde->bhte', qc * jnp.exp(b), state)
        diff = b[:, :, :, None, :] - b[:, :, None, :, :]
        decay = jnp.exp(jnp.where(causal[:, :, None], diff, -jnp.inf))
        amat = jnp.einsum('bhtd,bhsd,bhtsd->bhts', qc, kc, decay)
        intra = jnp.einsum('bhts,bhse->bhte', amat, vc)
        bL = b[:, :, -1]
        k_dec = kc * jnp.exp(bL[:, :, None, :] - b)
        new_state = jnp.exp(bL)[..., None] * state + jnp.einsum('bhsd,bhse->bhde', k_dec, vc)
        return new_state, inter + intra

    init = jnp.zeros((B, H, Dk, Dv), jnp.float32)
    xs = (_chunks(q, L), _chunks(k, L), _chunks(v, L), _chunks(log_a, L))
    _, os_ = lax.scan(step, init, xs)
    return _unchunk(os_)


def gla_layer(h, w_in, gate_w2, gate_b, norm_g, w_out, mem_k, mem_v):
    B, S, _ = h.shape
    H, Dk, Dv = GLA_HEADS, GLA_KEY_DIM, GLA_VALUE_DIM
    kw = H * Dk
    proj = h @ w_in
    q = proj[..., :kw]
    k = proj[..., kw:2 * kw]
    v = proj[..., 2 * kw:2 * kw + D_MODEL]
    g = proj[..., 2 * kw + D_MODEL:2 * kw + 2 * D_MODEL]
    z = proj[..., 2 * kw + 2 * D_MODEL:2 * kw + 2 * D_MODEL + GLA_GATE_RANK]
    xq = proj[..., 2 * kw + 2 * D_MODEL + GLA_GATE_RANK:]
    log_a = jax.nn.log_sigmoid((z @ gate_w2 + gate_b).astype(jnp.float32)) / GLA_TAU

    def heads(a, d):
        return a.reshape(B, S, H, d).transpose(0, 2, 1, 3).astype(jnp.float32)

    o = gla_chunkwise(heads(q, Dk), heads(k, Dk), heads(v, Dv), heads(log_a, Dk))
    o = rmsnorm(o.transpose(0, 2, 1, 3).astype(h.dtype), norm_g.reshape(H, Dv)).reshape(B, S, D_MODEL)
    o = o * jax.nn.silu(g)
    xo = memory_attention(xq, mem_k, mem_v)
    return jnp.concatenate([o, xo], axis=-1) @ w_out


def _layers_of(kind):
    return len(range(kind, DEPTH, N_MIXERS))


def setup_inputs(seed: int = 0) -> dict:
    key = jax.random.key(seed)
    keys = iter(jax.random.split(key, 40))

    def nrm(shape, scale):
        return jax.random.normal(next(keys), shape, jnp.float32) * scale

    def gain(shape):
        return 1.0 + nrm(shape, 0.05)

    nA, nB, nC, nD = (_layers_of(kd) for kd in range(N_MIXERS))
    D = D_MODEL
    x = nrm((BATCH, SEQ, D), 1.0)
    mem = nrm((BATCH, MEM_LEN, D), 1.0)
    norm_g = gain((DEPTH, 3, D))
    ffn_w_up = nrm((DEPTH, 2, D, 2 * D_FF), D ** -0.5)
    ffn_w_down = nrm((DEPTH, 2, D_FF, D), D_FF ** -0.5)
    mem_norm_g = gain((D,))
    mem_w_kv = nrm((DEPTH, D, 2 * X_WIDTH), D ** -0.5)
    pool_w_in = nrm((nA, D, POOL_IN), D ** -0.5)
    pool_w_group = nrm((nA, POOL_GROUPS, POOL_GROUP_DIM, POOL_GROUP_DIM), POOL_GROUP_DIM ** -0.5)
    pool_scale = gain((nA, D))
    pool_w_out = nrm((nA, MIX_OUT, D), MIX_OUT ** -0.5)
    diff_w_in = nrm((nB, D, DIFF_IN), D ** -0.5)
    diff_lambda = nrm((nB, 4, DIFF_HEAD_DIM), 0.1)
    diff_norm_g = gain((nB, 2 * DIFF_HEAD_DIM))
    diff_w_out = nrm((nB, MIX_OUT, D), MIX_OUT ** -0.5)
    mlstm_w_in = nrm((nC, D, MLSTM_IN), D ** -0.5)
    mlstm_conv_w = nrm((nC, MLSTM_CONV, 2 * D), MLSTM_CONV ** -0.5)
    i_bias = nrm((nC, MLSTM_HEADS), 0.1)
    f_bias = jnp.linspace(3.0, 6.0, MLSTM_HEADS, dtype=jnp.float32)[None] + nrm((nC, MLSTM_HEADS), 0.01)
    mlstm_gate_b = jnp.stack([i_bias, f_bias], axis=1)
    mlstm_norm_g = gain((nC, D))
    mlstm_w_out = nrm((nC, MIX_OUT, D), MIX_OUT ** -0.5)
    gla_w_in = nrm((nD, D, GLA_IN), D ** -0.5)
    gla_gate_w2 = nrm((nD, GLA_GATE_RANK, GLA_HEADS * GLA_KEY_DIM), GLA_GATE_RANK ** -0.5)
    gla_gate_b = nrm((nD, GLA_HEADS * GLA_KEY_DIM), 0.1)
    gla_norm_g = gain((nD, D))
    gla_w_out = nrm((nD, MIX_OUT, D), MIX_OUT ** -0.5)
    final_norm_g = gain((D,))
    return {'x': x, 'mem': mem, 'norm_g': norm_g, 'ffn_w_up': ffn_w_up, 'ffn_w_down': ffn_w_down,
            'mem_norm_g': mem_norm_g, 'mem_w_kv': mem_w_kv,
            'pool_w_in': pool_w_in, 'pool_w_group': pool_w_group, 'pool_scale': pool_scale, 'pool_w_out': pool_w_out,
            'diff_w_in': diff_w_in, 'diff_lambda': diff_lambda, 'diff_norm_g': diff_norm_g, 'diff_w_out': diff_w_out,
            'mlstm_w_in': mlstm_w_in, 'mlstm_conv_w': mlstm_conv_w, 'mlstm_gate_b': mlstm_gate_b,
            'mlstm_norm_g': mlstm_norm_g, 'mlstm_w_out': mlstm_w_out,
            'gla_w_in': gla_w_in, 'gla_gate_w2': gla_gate_w2, 'gla_gate_b': gla_gate_b,
            'gla_norm_g': gla_norm_g, 'gla_w_out': gla_w_out, 'final_norm_g': final_norm_g}


def reference(x, mem, norm_g, ffn_w_up, ffn_w_down, mem_norm_g, mem_w_kv,
              pool_w_in, pool_w_group, pool_scale, pool_w_out,
              diff_w_in, diff_lambda, diff_norm_g, diff_w_out,
              mlstm_w_in, mlstm_conv_w, mlstm_gate_b, mlstm_norm_g, mlstm_w_out,
              gla_w_in, gla_gate_w2, gla_gate_b, gla_norm_g, gla_w_out, final_norm_g):
    B, M, _ = mem.shape
    mem_n = rmsnorm(mem, mem_norm_g)
    slopes = alibi_slopes(DIFF_HEADS)
    for i in range(DEPTH):
        kind, j = i % N_MIXERS, i // N_MIXERS
        kv = mem_n @ mem_w_kv[i]
        mem_k = kv[..., :X_WIDTH].reshape(B, M, X_HEADS, X_HEAD_DIM)
        mem_v = kv[..., X_WIDTH:].reshape(B, M, X_HEADS, X_HEAD_DIM)
        x = x + 0.5 * swiglu(rmsnorm(x, norm_g[i, 0]), ffn_w_up[i, 0], ffn_w_down[i, 0])
        h = rmsnorm(x, norm_g[i, 1])
        if kind == 0:
            mix = pool_layer(h, pool_w_in[j], pool_w_group[j], pool_scale[j], pool_w_out[j], mem_k, mem_v)
        elif kind == 1:
            mix = diff_layer(h, diff_w_in[j], diff_lambda[j], diff_norm_g[j], diff_w_out[j], mem_k, mem_v, slopes, i)
        elif kind == 2:
            mix = mlstm_layer(h, mlstm_w_in[j], mlstm_conv_w[j], mlstm_gate_b[j], mlstm_norm_g[j], mlstm_w_out[j],
                              mem_k, mem_v)
        else:
            mix = gla_layer(h, gla_w_in[j], gla_gate_w2[j], gla_gate_b[j], gla_norm_g[j], gla_w_out[j], mem_k, mem_v)
        x = x + mix
        x = x + 0.5 * swiglu(rmsnorm(x, norm_g[i, 2]), ffn_w_up[i, 1], ffn_w_down[i, 1])
    return rmsnorm(x, final_norm_g)
```

```python
import numpy as np
import concourse.bass as bass
import concourse.mybir as mybir
from concourse.bass_utils import run_bass_kernel_spmd

F32 = mybir.dt.float32
BF16 = mybir.dt.bfloat16
AF = mybir.ActivationFunctionType
ALU = mybir.AluOpType
AX = mybir.AxisListType


class Buf:
    __slots__ = ("name", "lw", "rd")

    def __init__(self, name):
        self.name = name
        self.lw = None
        self.rd = {}


class Sched:
    def __init__(self, nc):
        self.nc = nc
        self.prog = {e: [] for e in ("pe", "act", "dve", "pool", "sp")}
        self.sems = {}
        self.waited = {e: {} for e in self.prog}
        self.ninst = {e: 0 for e in self.prog}

    def _sem(self, key):
        if key not in self.sems:
            self.sems[key] = [self.nc.alloc_semaphore("s_" + key), 0]
        return self.sems[key]

    def _deps(self, eng, reads, writes):
        need = {}

        def add(tok, skip_same):
            if tok is None:
                return
            key, val, src = tok
            if skip_same and src == eng:
                return
            if need.get(key, 0) < val:
                need[key] = val

        for b in reads:
            add(b.lw, eng == "pe")
        for b in writes:
            add(b.lw, True)
            for t in b.rd.values():
                add(t, True)
        waits = []
        w = self.waited[eng]
        for key, val in need.items():
            if w.get(key, 0) < val:
                w[key] = val
                waits.append((self.sems[key][0], val))
        return waits

    def op(self, eng, fn, reads=(), writes=()):
        waits = self._deps(eng, reads, writes)
        s = self._sem(eng)
        s[1] += 1
        tok = (eng, s[1], eng)
        for b in reads:
            b.rd[eng] = tok
        for b in writes:
            b.lw = tok
            b.rd = {}
        sem = s[0]

        def run(e):
            for (h, v) in waits:
                e.wait_ge(h, v)
            fn(e).then_inc(sem, 1)

        self.prog[eng].append(run)
        self.ninst[eng] += 1

    def dma(self, q, pairs, reads=(), writes=(), key=None):
        waits = self._deps(q, reads, writes)
        s = self._sem(key)
        if s[1] > 0 and self.waited[q].get(key, 0) < s[1]:
            self.waited[q][key] = s[1]
            waits.append((s[0], s[1]))
        s[1] += 16 * len(pairs)
        tok = (key, s[1], None)
        for b in reads:
            b.rd[key] = tok
        for b in writes:
            b.lw = tok
            b.rd = {}
        sem = s[0]

        def run(e):
            for (h, v) in waits:
                e.wait_ge(h, v)
            for (o, i) in pairs:
                e.dma_start(out=o, in_=i).then_inc(sem, 16)

        self.prog[q].append(run)
        self.ninst[q] += len(pairs)

    def finish(self, bufs):
        waits = self._deps("sp", bufs, ())

        def run(e):
            for (h, v) in waits:
                e.wait_ge(h, v)

        self.prog["sp"].append(run)

    def emit(self):
        nc = self.nc
        prog = self.prog
        with nc.Block() as block:
            @block.tensor
            def _(e):
                for f in prog["pe"]:
                    f(e)

            @block.scalar
            def _(e):
                for f in prog["act"]:
                    f(e)

            @block.vector
            def _(e):
                for f in prog["dve"]:
                    f(e)

            @block.gpsimd
            def _(e):
                for f in prog["pool"]:
                    f(e)

            @block.sync
            def _(e):
                for f in prog["sp"]:
                    f(e)


S_ = 4096
D_ = 1024
DFF = 2816
NT = 512
NTL = S_ // NT
NCH = D_ // 128
NJ = DFF // 128
DEPTH = 4
MEMLEN = 256


def bl(name, n):
    return [Buf(f"{name}{i}") for i in range(n)]


class Prog:
    def __init__(self, nc):
        self.nc = nc
        self.sc = Sched(nc)
        self.dram = {}
        self.uid = 0
        self.ps = [nc.alloc_psum_tensor(f"psb{i}", [128, 512], F32) for i in range(8)]
        self.psB = bl("psb", 8)
        self.arena_base = None
        self.arena_off = 0
        self.wkey = 0

    def din(self, name, shape, dtype=F32):
        t = self.nc.dram_tensor(name, list(shape), dtype, kind="ExternalInput")
        self.dram[name] = t
        return t

    def dint(self, name, shape, dtype=F32):
        t = self.nc.dram_tensor(name, list(shape), dtype, kind="Internal")
        self.dram[name] = t
        return t

    def dout(self, name, shape, dtype=F32):
        t = self.nc.dram_tensor(name, list(shape), dtype, kind="ExternalOutput")
        self.dram[name] = t
        return t

    def const(self, name, shape, dtype):
        return self.nc.alloc_sbuf_tensor(name, list(shape), dtype)

    def arena_start(self):
        nc = self.nc
        total = nc.SBUF_PARTITION_SIZE_BYTES
        rem = nc.sbuf_bytes_remaining
        self.arena_base = ((total - rem + 63) // 64) * 64
        self.arena_end = total
        self.arena_off = self.arena_base

    def stage_begin(self):
        self.sc.barrier()
        self.arena_off = self.arena_base

    def alloc(self, name, shape, dtype):
        esz = 4 if dtype == F32 else 2
        nbytes = esz
        for s in shape[1:]:
            nbytes *= s
        off = self.arena_off
        self.arena_off = ((off + nbytes + 63) // 64) * 64
        assert self.arena_off <= self.arena_end, (name, self.arena_off, self.arena_end)
        self.uid += 1
        return self.nc.alloc_sbuf_tensor_at(f"{name}_{self.uid}", list(shape), dtype, offset=off)

    def next_wkey(self):
        self.wkey = (self.wkey + 1) % 6
        return f"w{self.wkey}"


def _barrier(self):
    snap = {k: v[1] for k, v in self.sems.items() if v[1] > 0}
    for eng in self.prog:
        waits = []
        w = self.waited[eng]
        for key, val in snap.items():
            if key == eng:
                continue
            if w.get(key, 0) < val:
                w[key] = val
                waits.append((self.sems[key][0], val))
        if waits:
            def run(e, waits=waits):
                for (h, v) in waits:
                    e.wait_ge(h, v)
            self.prog[eng].append(run)


Sched.barrier = _barrier


def rmsnorm_tile(P, X, XB, gcol, h, HB, sq, SQB, tmp, TB, eps=1e-6, nch=NCH, ones=None, n=NT,
                 split_pool=False):
    sc = P.sc
    pm = P.ps[7]
    PMB = P.psB[7]
    ones = P.ones_mean if ones is None else ones
    for c in range(nch):
        sc.op("act", lambda e, c=c: e.activation(out=sq[:, c, :n], in_=X[:, c, :n], func=AF.Square),
              reads=[XB[c]], writes=[SQB[c]])

    def mm(e):
        r = None
        for c in range(nch):
            r = e.matmul(pm[:, :n], ones[:, :], sq[:, c, :n], start=(c == 0), stop=(c == nch - 1))
        return r
    sc.op("pe", mm, reads=SQB[:nch], writes=[PMB])
    sc.op("act", lambda e: e.activation(out=tmp[:, 0, :n], in_=pm[:, :n], func=AF.Ln, bias=P.epsc(eps), scale=1.0),
          reads=[PMB], writes=[TB[0]])
    sc.op("act", lambda e: e.activation(out=tmp[:, 1, :n], in_=tmp[:, 0, :n], func=AF.Exp, scale=-0.5),
          reads=[TB[0]], writes=[TB[1]])
    for c in range(nch):
        eng = "pool" if (split_pool and c % 2 == 1) else "dve"
        gap = gcol(c)
        sc.op(eng, lambda e, c=c, gap=gap: e.scalar_tensor_tensor(out=h[:, c, :n], in0=X[:, c, :n], scalar=gap,
                                                                  in1=tmp[:, 1, :n], op0=ALU.mult, op1=ALU.mult),
              reads=[XB[c], TB[1]], writes=[HB[c]])


def ffn_stage(P, li, fi, xin, xout, XinB, XoutB):
    sc = P.sc
    P.stage_begin()
    wup = P.alloc("wup", [128, NCH, 2 * DFF], BF16)
    wdn = P.alloc("wdn", [128, NJ, D_], BF16)
    Xs = [P.alloc("X", [128, NCH, NT], F32) for _ in range(2)]
    h = P.alloc("h", [128, NCH, NT], BF16)
    act = P.alloc("act", [128, NJ, NT], BF16)
    tmp = P.alloc("tmp", [128, 2, NT], F32)
    sg = [P.alloc("sg", [128, NT], BF16) for _ in range(2)]
    XB = [bl("X", NCH) for _ in range(2)]
    HB = bl("h", NCH)
    AB = bl("act", NJ)
    TB = bl("tmp", 2)
    SGB = bl("sg", 2)
    wu_d = P.dram["ffn_w_up"].ap()[li, fi].rearrange("(c p) f -> p c f", p=128)
    wd_d = P.dram["ffn_w_down"].ap()[li, fi].rearrange("(j p) d -> p j d", p=128)
    WUB = bl("wu", 11)
    WDB = bl("wd", 11)
    for jb in range(11):
        c0 = jb * 256
        sc.dma("pool", [(wup[:, :, c0:c0 + 256], wu_d[:, :, c0:c0 + 256]),
                        (wup[:, :, DFF + c0:DFF + c0 + 256], wu_d[:, :, DFF + c0:DFF + c0 + 256])],
               writes=[WUB[jb]], key=P.next_wkey())
    for jb in range(11):
        sc.dma("pool", [(wdn[:, 2 * jb:2 * jb + 2, :], wd_d[:, 2 * jb:2 * jb + 2, :])],
               writes=[WDB[jb]], key=P.next_wkey())
    xin_r = xin.ap().rearrange("(c p) n -> p c n", p=128)
    xout_r = xout.ap().rearrange("(c p) n -> p c n", p=128)
    gt = P.gtab
    gbase = (li * 3 + (0 if fi == 0 else 2)) * NCH

    def load(t):
        s = t % 2
        sc.dma("sp", [(Xs[s][:, :, :], xin_r[:, :, t * NT:(t + 1) * NT])], reads=[XinB[t]], writes=XB[s],
               key=f"xld{s}")

    load(0)
    for t in range(NTL):
        s = t % 2
        X = Xs[s]
        if t + 1 < NTL:
            load(t + 1)
        rmsnorm_tile(P, X, XB[s], lambda c: gt[:, gbase + c:gbase + c + 1], h, HB, act, AB, tmp, TB)
        for j in range(NJ):
            k = j % 2
            pg, pu = P.ps[2 * k], P.ps[2 * k + 1]

            def mm(e, j=j, pg=pg, pu=pu):
                for c in range(NCH):
                    e.matmul(pg[:, :], wup[:, c, j * 128:(j + 1) * 128], h[:, c, :], start=(c == 0), stop=(c == NCH - 1))
                r = None
                for c in range(NCH):
                    r = e.matmul(pu[:, :], wup[:, c, DFF + j * 128:DFF + (j + 1) * 128], h[:, c, :], start=(c == 0),
                                 stop=(c == NCH - 1))
                return r
            sc.op("pe", mm, reads=HB + [WUB[j // 2]], writes=[P.psB[2 * k], P.psB[2 * k + 1]])
            sc.op("act", lambda e, k=k, pg=pg: e.activation(out=sg[k][:, :], in_=pg[:, :], func=AF.Silu),
                  reads=[P.psB[2 * k]], writes=[SGB[k]])
            sc.op("dve", lambda e, k=k, pu=pu, j=j: e.tensor_tensor(out=act[:, j, :], in0=sg[k][:, :], in1=pu[:, :], op=ALU.mult),
                  reads=[SGB[k], P.psB[2 * k + 1]], writes=[AB[j]])
        for c in range(NCH):
            k = 4 + (c % 2)
            pd = P.ps[k]

            def mm2(e, c=c, pd=pd):
                r = None
                for j in range(NJ):
                    r = e.matmul(pd[:, :], wdn[:, j, c * 128:(c + 1) * 128], act[:, j, :], start=(j == 0), stop=(j == NJ - 1))
                return r
            sc.op("pe", mm2, reads=AB + WDB, writes=[P.psB[k]])
            sc.op("dve", lambda e, c=c, pd=pd, X=X: e.scalar_tensor_tensor(out=X[:, c, :], in0=pd[:, :], scalar=0.5, in1=X[:, c, :],
                                                                      op0=ALU.mult, op1=ALU.add),
                  reads=[P.psB[k], XB[s][c]], writes=[XB[s][c]])
        sc.dma("sp", [(xout_r[:, :, t * NT:(t + 1) * NT], X[:, :, :])], reads=XB[s], writes=[XoutB[t]], key=f"xst{s}")


def final_stage(P, xin, xout, XinB, XoutB, norm=True):
    sc = P.sc
    P.stage_begin()
    Xs = [P.alloc("X", [128, NCH, NT], F32) for _ in range(2)]
    Ys = [P.alloc("Y", [128, NCH, NT], F32) for _ in range(2)]
    sq = P.alloc("sq", [128, NCH, NT], BF16)
    tmp = P.alloc("tmp", [128, 2, NT], F32)
    XB = [bl("X", NCH) for _ in range(2)]
    YB = [bl("Y", NCH) for _ in range(2)]
    SQB = bl("sq", NCH)
    TB = bl("tmp", 2)
    xin_r = xin.ap().rearrange("(c p) n -> p c n", p=128)
    xout_r = xout.ap().rearrange("(c p) n -> p c n", p=128)
    gbase = DEPTH * 3 * NCH
    gt = P.gtab
    for t in range(NTL):
        s = t % 2
        sc.dma("sp", [(Xs[s][:, :, :], xin_r[:, :, t * NT:(t + 1) * NT])], reads=[XinB[t]], writes=XB[s], key=f"xld{s}")
        if norm:
            rmsnorm_tile(P, Xs[s], XB[s], lambda c: gt[:, gbase + c:gbase + c + 1], Ys[s], YB[s], sq, SQB, tmp, TB)
            sc.dma("sp", [(xout_r[:, :, t * NT:(t + 1) * NT], Ys[s][:, :, :])], reads=YB[s], writes=[XoutB[t]], key=f"xst{s}")
        else:
            sc.dma("sp", [(xout_r[:, :, t * NT:(t + 1) * NT], Xs[s][:, :, :])], reads=XB[s], writes=[XoutB[t]], key=f"xst{s}")


WEIGHT_SHAPES = {
    "ffn_w_up": (4, 2, 1024, 5632), "ffn_w_down": (4, 2, 2816, 1024), "mem_w_kv": (4, 1024, 1024),
    "pool_w_in": (1, 1024, 1536), "pool_w_group": (1, 4, 256, 256), "pool_w_out": (1, 1536, 1024),
    "diff_w_in": (1, 1024, 3584), "diff_w_out": (1, 1536, 1024),
    "mlstm_w_in": (1, 1024, 4616), "mlstm_w_out": (1, 1536, 1024),
    "gla_w_in": (1, 1024, 3600), "gla_w_out": (1, 1536, 1024), "gla_gate_w2": (1, 16, 512),
}
VT = {}
_off = 0
for _n, _w in [("norm", 14 * NCH), ("pool_scale", 8), ("diff_norm", 1), ("mlstm_norm", 8), ("gla_norm", 8),
               ("conv_w", 64), ("gla_gate_b", 4), ("mlstm_gate_b", 8), ("diff_lambda", 256)]:
    VT[_n] = _off
    _off += _w
NV = _off
GI_FINAL = 12
GI_MEM = 13
DBG = {}
MIX_NAMES = ["pool", "diff", "mlstm", "gla"]
XQ_OFF = [1024, 3072, 4104, 3088]
IN_W = [1536, 3584, 4616, 3600]


def load_w(P, dst, src, key_bufs, nsplit, axis=2):
    n = dst.shape[axis]
    step = (n + nsplit - 1) // nsplit
    bufs = []
    for i in range(nsplit):
        a, b = i * step, min(n, (i + 1) * step)
        if a >= b:
            break
        B = Buf(f"{key_bufs}{i}")
        if axis == 2:
            P.sc.dma("pool", [(dst[:, :, a:b], src[:, :, a:b])], writes=[B], key=P.next_wkey())
        else:
            P.sc.dma("pool", [(dst[:, a:b, :], src[:, a:b, :])], writes=[B], key=P.next_wkey())
        bufs.append(B)
    return bufs


def mixer_stage(P, li, xin, xout, XinB, XoutB):
    sc = P.sc
    kind = li % 4
    name = MIX_NAMES[kind]
    P.stage_begin()
    gt = P.vtab
    w_in_d = P.dram[name + "_w_in"].ap()[0].rearrange("(c p) f -> p c f", p=128)
    w_out_d = P.dram[name + "_w_out"].ap()[0].rearrange("(k p) d -> p k d", p=128)
    wkv_d = P.dram["mem_w_kv"].ap()[li].rearrange("(c p) f -> p c f", p=128)
    xin_r = xin.ap().rearrange("(c p) n -> p c n", p=128)
    xout_r = xout.ap().rearrange("(c p) n -> p c n", p=128)
    cat_r = P.dram["catd"].ap().rearrange("(k p) n -> p k n", p=128)
    CatMixB, CatXoB = bl("catmix", NTL), bl("catxo", NTL)
    hall = P.alloc("hall", [128, NCH, S_], BF16)
    HallB = [bl("hall", NCH) for _ in range(NTL)]
    PRE_WOUT = kind in DBG.get("pre_wout", (0, 3))
    if PRE_WOUT:
        wout = P.alloc("wout", [128, 12, D_], BF16)
    mark = P.arena_off
    wkv = P.alloc("wkv", [128, NCH, 1024], BF16)
    wxq = P.alloc("wxq", [128, NCH, 512], BF16)
    memK = P.alloc("memK", [128, 4, MEMLEN], BF16)
    memV = P.alloc("memV", [128, 2, 512], BF16)
    Xs = [P.alloc("X", [128, NCH, NT], F32) for _ in range(2)]
    sq = P.alloc("sq", [128, NCH, NT], BF16)
    tmp = P.alloc("tmp", [128, 2, NT], F32)
    xq = P.alloc("xq", [128, 4, NT], BF16)
    pT = [P.alloc("pT", [128, 2, NT], BF16) for _ in range(2)]
    rden = [P.alloc("rden", [128, NT], F32) for _ in range(2)]
    xo = [P.alloc("xo", [128, 4, NT], BF16) for _ in range(2)]
    XB = [bl("X", NCH) for _ in range(2)]
    SQB, TB = bl("sq", NCH), bl("tmp", 2)
    XQB = bl("xq", 4)
    PTB, RDB = bl("pT", 2), bl("rden", 2)
    XOB = [bl("xo", 4) for _ in range(2)]
    WKVB = load_w(P, wkv, wkv_d, "wkv", 2)
    WXQB = load_w(P, wxq, w_in_d[:, :, XQ_OFF[kind]:XQ_OFF[kind] + 512], "wxq", 1)
    if PRE_WOUT:
        WOB = load_w(P, wout, w_out_d, "wout", 3, axis=1)
    MKB, MVB = Buf("memK"), Buf("memV")
    mem_n = P.mem_n
    for hh in range(4):
        bk = P.bank()

        def mm(e, hh=hh, bk=bk):
            r = None
            for k in range(NCH):
                r = e.matmul(P.ps[bk][:, :MEMLEN], wkv[:, k, hh * 128:(hh + 1) * 128], mem_n[:, k, :], start=(k == 0), stop=(k == NCH - 1))
            return r
        sc.op("pe", mm, reads=[WKVB[0], P.MemNB], writes=[P.psB[bk]])
        sc.op("act", lambda e, hh=hh, bk=bk: e.activation(out=memK[:, hh, :], in_=P.ps[bk][:, :MEMLEN], func=AF.Copy),
              reads=[P.psB[bk]], writes=[MKB])
    for mc in range(2):
        bk = P.bank()

        def mm(e, mc=mc, bk=bk):
            r = None
            for k in range(NCH):
                r = e.matmul(P.ps[bk][:, :], mem_n[:, k, mc * 128:(mc + 1) * 128], wkv[:, k, 512:1024], start=(k == 0), stop=(k == NCH - 1))
            return r
        sc.op("pe", mm, reads=[WKVB[1], P.MemNB], writes=[P.psB[bk]])
        sc.op("act", lambda e, mc=mc, bk=bk: e.activation(out=memV[:, mc, :], in_=P.ps[bk][:, :], func=AF.Copy),
              reads=[P.psB[bk]], writes=[MVB])
    gbase = VT["norm"] + (li * 3 + 1) * NCH

    def loadx(t, Xs=Xs, XB=XB):
        s = t % 2
        sc.dma("sp", [(Xs[s][:, :, :], xin_r[:, :, t * NT:(t + 1) * NT])], reads=[XinB[t]], writes=XB[s], key=f"xld{s}")
    def normx(t):
        rmsnorm_tile(P, Xs[t % 2], XB[t % 2], lambda c: gt[:, gbase + c:gbase + c + 1], hall[:, :, t * NT:(t + 1) * NT], HallB[t], sq, SQB, tmp, TB)
    loadx(0)
    loadx(1)
    normx(0)
    for t in range(NTL):
        s = t % 2
        if t + 1 < NTL:
            normx(t + 1)
        if t + 2 < NTL:
            loadx(t + 2)
        hv = hall[:, :, t * NT:(t + 1) * NT]
        for hh in range(4):
            bk = P.bank()

            def mm(e, hh=hh, bk=bk, hv=hv):
                r = None
                for k in range(NCH):
                    r = e.matmul(P.ps[bk][:, :], wxq[:, k, hh * 128:(hh + 1) * 128], hv[:, k, :], start=(k == 0), stop=(k == NCH - 1))
                return r
            sc.op("pe", mm, reads=HallB[t] + WXQB, writes=[P.psB[bk]])
            sc.op("act", lambda e, hh=hh, bk=bk: e.mul(out=xq[:, hh, :], in_=P.ps[bk][:, :], mul=128.0 ** -0.5),
                  reads=[P.psB[bk]], writes=[XQB[hh]])
        for hh in range(4):
            k2 = hh % 2
            for mc in range(2):
                bk = P.bank()
                sc.op("pe", lambda e, hh=hh, mc=mc, bk=bk: e.matmul(P.ps[bk][:, :], memK[:, hh, mc * 128:(mc + 1) * 128], xq[:, hh, :], start=True, stop=True),
                      reads=[MKB, XQB[hh]], writes=[P.psB[bk]])
                sc.op("act", lambda e, k2=k2, mc=mc, bk=bk: e.activation(out=pT[k2][:, mc, :], in_=P.ps[bk][:, :], func=AF.Exp),
                      reads=[P.psB[bk]], writes=[PTB[k2]])
            bo, bd = P.bank(), P.bank()

            def mm(e, hh=hh, k2=k2, bo=bo, bd=bd):
                e.matmul(P.ps[bo][:, :], memV[:, 0, hh * 128:(hh + 1) * 128], pT[k2][:, 0, :], start=True, stop=False)
                e.matmul(P.ps[bo][:, :], memV[:, 1, hh * 128:(hh + 1) * 128], pT[k2][:, 1, :], start=False, stop=True)
                e.matmul(P.ps[bd][:, :], P.ones_bf[:, :], pT[k2][:, 0, :], start=True, stop=False)
                return e.matmul(P.ps[bd][:, :], P.ones_bf[:, :], pT[k2][:, 1, :], start=False, stop=True)
            sc.op("pe", mm, reads=[MVB, PTB[k2]], writes=[P.psB[bo], P.psB[bd]])
            sc.op("act", lambda e, k2=k2, bd=bd: e.activation(out=rden[k2][:, :], in_=P.ps[bd][:, :], func=AF.Ln), reads=[P.psB[bd]], writes=[RDB[k2]])
            sc.op("act", lambda e, k2=k2: e.activation(out=rden[k2][:, :], in_=rden[k2][:, :], func=AF.Exp, scale=-1.0), reads=[RDB[k2]], writes=[RDB[k2]])
            sc.op("dve", lambda e, k2=k2, bo=bo, hh=hh, s=s: e.tensor_tensor(out=xo[s][:, hh, :], in0=P.ps[bo][:, :], in1=rden[k2][:, :], op=ALU.mult),
                  reads=[P.psB[bo], RDB[k2]], writes=[XOB[s][hh]])
        sc.dma("sp", [(cat_r[:, 8:12, t * NT:(t + 1) * NT], xo[s][:, :, :])], reads=XOB[s], writes=[CatXoB[t]], key=f"xost{s}")
    sc.barrier()
    P.arena_off = mark
    [pool_mix, diff_mix, mlstm_mix, gla_mix][kind](P, li, hall, HallB, w_in_d, cat_r, CatMixB)
    sc.barrier()
    P.arena_off = mark
    if not PRE_WOUT:
        wout = P.alloc("wout", [128, 12, D_], BF16)
        WOB = load_w(P, wout, w_out_d, "wout", 3, axis=1)
    Xs = [P.alloc("X", [128, NCH, NT], F32) for _ in range(2)]
    cats = [P.alloc("cat", [128, 12, NT], BF16) for _ in range(2)]
    XB = [bl("X", NCH) for _ in range(2)]
    CB = bl("cat", 2)

    def load3(t):
        s = t % 2
        sc.dma("sp", [(Xs[s][:, :, :], xin_r[:, :, t * NT:(t + 1) * NT])], reads=[XinB[t]], writes=XB[s], key=f"xld{s}")
        sc.dma("sp", [(cats[s][:, :, :], cat_r[:, :, t * NT:(t + 1) * NT])], reads=[CatMixB[t], CatXoB[t]], writes=[CB[s]], key=f"cld{s}")
    load3(0)
    for t in range(NTL):
        s = t % 2
        if t + 1 < NTL:
            load3(t + 1)
        for c in range(NCH):
            bk = P.bank()

            def mm(e, c=c, bk=bk, s=s):
                r = None
                for k in range(12):
                    r = e.matmul(P.ps[bk][:, :], wout[:, k, c * 128:(c + 1) * 128], cats[s][:, k, :], start=(k == 0), stop=(k == 11))
                return r
            sc.op("pe", mm, reads=[CB[s]] + WOB, writes=[P.psB[bk]])
            sc.op("dve", lambda e, c=c, bk=bk, s=s: e.tensor_tensor(out=Xs[s][:, c, :], in0=P.ps[bk][:, :], in1=Xs[s][:, c, :], op=ALU.add),
                  reads=[P.psB[bk], XB[s][c]], writes=[XB[s][c]])
        sc.dma("sp", [(xout_r[:, :, t * NT:(t + 1) * NT], Xs[s][:, :, :])], reads=XB[s], writes=[XoutB[t]], key=f"xst{s}")


def pool_mix(P, li, hall, HallB, w_in_d, cat_r, CatMixB):
    sc = P.sc
    gt = P.vtab
    win = P.alloc("win", [128, NCH, 1024], BF16)
    wg = P.alloc("wg", [128, 8, 256], BF16)
    WINB = load_w(P, win, w_in_d[:, :, 0:1024], "win", 2)
    wg_d = P.dram["pool_w_group"].ap()[0].rearrange("g (i p) o -> p (g i) o", p=128)
    WGB = load_w(P, wg, wg_d, "wg", 1)
    inv0 = P.alloc("inv0", [128, 4, NT], F32)
    INVB = Buf("inv0")
    sc.dma("sp", [(inv0[:, :, :], P.dram["inv0"].ap())], writes=[INVB], key="cst")
    uext = [P.alloc("uext", [128, NCH, 16 + NT], F32) for _ in range(2)]
    wa = P.alloc("wa", [128, 16 + NT], F32)
    wb = P.alloc("wb", [128, 16 + NT], F32)
    pooled = P.alloc("pooled", [128, NCH, NT], BF16)
    mix = [P.alloc("mix", [128, NCH, NT], BF16) for _ in range(2)]
    UB = [bl("u", NCH) for _ in range(2)]
    WAB, WBB = Buf("wa"), Buf("wb")
    PB = bl("pooled", NCH)
    MB = [bl("mix", NCH) for _ in range(2)]
    W2 = NT + 16
    for t in range(NTL):
        s = t % 2
        hv = hall[:, :, t * NT:(t + 1) * NT]
        for c in range(NCH):
            bk = P.bank()
            if t == 0:
                sc.op("dve", lambda e, c=c: e.memset(uext[0][:, c, 0:16], 0.0), writes=[UB[0][c]])
            else:
                sc.op("dve", lambda e, c=c, s=s: e.tensor_copy(out=uext[s][:, c, 0:16], in_=uext[1 - s][:, c, NT:NT + 16]),
                      reads=[UB[1 - s][c]], writes=[UB[s][c]])

            def mm(e, c=c, bk=bk, hv=hv):
                r = None
                for k in range(NCH):
                    r = e.matmul(P.ps[bk][:, :], win[:, k, c * 128:(c + 1) * 128], hv[:, k, :], start=(k == 0), stop=(k == NCH - 1))
                return r
            sc.op("pe", mm, reads=HallB[t] + WINB, writes=[P.psB[bk]])
            sc.op("act", lambda e, c=c, bk=bk, s=s: e.activation(out=uext[s][:, c, 16:W2], in_=P.ps[bk][:, :], func=AF.Copy),
                  reads=[P.psB[bk]], writes=[UB[s][c]])
            g = c // 2
            w = 2 ** (g + 1)
            u = uext[s]
            src_ap = lambda lo, hi, c=c, u=u: u[:, c, lo:hi]
            cur = None
            bufs = [(wa, WAB), (wb, WBB)]
            for i in range(g + 1):
                sh = 2 ** i
                lo = 2 ** (i + 1) - 1
                dst, DB = bufs[i % 2]
                if i == 0:
                    sc.op("dve", lambda e, dst=dst, lo=lo, sh=sh, c=c, u=u: e.tensor_tensor(out=dst[:, lo:W2], in0=u[:, c, lo:W2], in1=u[:, c, lo - sh:W2 - sh], op=ALU.add),
                          reads=[UB[s][c]], writes=[DB])
                else:
                    srcb, SB = bufs[(i - 1) % 2]
                    sc.op("dve", lambda e, dst=dst, srcb=srcb, lo=lo, sh=sh: e.tensor_tensor(out=dst[:, lo:W2], in0=srcb[:, lo:W2], in1=srcb[:, lo - sh:W2 - sh], op=ALU.add),
                          reads=[SB], writes=[DB])
                cur = (dst, DB)
            dst, DB = cur
            if t == 0:
                sc.op("dve", lambda e, dst=dst, g=g: e.tensor_tensor(out=dst[:, 16:W2], in0=dst[:, 16:W2], in1=inv0[:, g, :], op=ALU.mult),
                      reads=[DB, INVB], writes=[DB])
                sc.op("dve", lambda e, dst=dst, c=c, u=u: e.tensor_tensor(out=pooled[:, c, :], in0=dst[:, 16:W2], in1=u[:, c, 16:W2], op=ALU.subtract),
                      reads=[DB, UB[s][c]], writes=[PB[c]])
            else:
                sc.op("dve", lambda e, dst=dst, c=c, u=u, w=w: e.scalar_tensor_tensor(out=pooled[:, c, :], in0=dst[:, 16:W2], scalar=1.0 / w, in1=u[:, c, 16:W2],
                                                                                   op0=ALU.mult, op1=ALU.subtract),
                      reads=[DB, UB[s][c]], writes=[PB[c]])
        for co in range(NCH):
            g, o = co // 2, co % 2
            bk = P.bank()

            def mm(e, g=g, o=o, bk=bk):
                e.matmul(P.ps[bk][:, :], wg[:, g * 2, o * 128:(o + 1) * 128], pooled[:, g * 2, :], start=True, stop=False)
                return e.matmul(P.ps[bk][:, :], wg[:, g * 2 + 1, o * 128:(o + 1) * 128], pooled[:, g * 2 + 1, :], start=False, stop=True)
            sc.op("pe", mm, reads=[PB[g * 2], PB[g * 2 + 1]] + WGB, writes=[P.psB[bk]])
            col = VT["pool_scale"] + co
            sc.op("act", lambda e, co=co, bk=bk, s=s, col=col: e.activation(out=mix[s][:, co, :], in_=P.ps[bk][:, :], func=AF.Copy, scale=gt[:, col:col + 1]),
                  reads=[P.psB[bk], P.ConstB], writes=[MB[s][co]])
        sc.dma("sp", [(cat_r[:, 0:8, t * NT:(t + 1) * NT], mix[s][:, :, :])], reads=MB[s], writes=[CatMixB[t]], key=f"mixst{s}")


def diff_mix(P, li, hall, HallB, w_in_d, cat_r, CatMixB):
    import math
    sc = P.sc
    gt = P.vtab
    H = 8
    lam_init = 0.8 - 0.6 * math.exp(-0.3 * li)
    win = P.alloc("win", [128, NCH, 3072], BF16)
    WINB = load_w(P, win, w_in_d[:, :, 0:3072], "win", 6)
    allW = list(WINB)
    maskd = P.alloc("maskd", [128, 4, NT], BF16)
    ident = P.alloc("ident", [128, 128], BF16)
    CMB = Buf("dconst")
    sc.dma("pool", [(maskd[:, :, :], P.dram["maskd"].ap()), (ident[:, :], P.dram["ident"].ap())], writes=[CMB], key="cstp")
    qa = P.alloc("qa", [128, 2, S_], BF16)
    ka = P.alloc("ka", [128, 2, S_], BF16)
    v = P.alloc("v", [128, 32, 128], BF16)
    QB, KB, VB = bl("qa", NTL), bl("ka", NTL), bl("v", NTL)
    QRB, KRB = Buf("qrows"), Buf("krows")
    pT = [P.alloc("pT", [128, NT], BF16) for _ in range(4)]
    PTB = bl("pT", 4)
    rd = [P.alloc("rd", [128, NT], F32) for _ in range(2)]
    tt = [P.alloc("tt", [128, NT], F32) for _ in range(2)]
    osb = P.alloc("osb", [128, NT], F32)
    sqo = P.alloc("sqo", [128, NT], BF16)
    lnr = P.alloc("lnr", [128, 2, NT], F32)
    mixh = [P.alloc("mixh", [128, NT], BF16) for _ in range(2)]
    RDB, TTB, OSB, SQOB, LNB, MXB = bl("rd", 2), bl("tt", 2), Buf("osb"), Buf("sqo"), bl("lnr", 2), bl("mixh", 2)
    sm = P.alloc("sm", [128, 8], F32)
    pr = P.alloc("pr", [128, 2, 64], F32)
    SMB, PRB = Buf("sm"), Buf("pr")
    lo = VT["diff_lambda"]
    sc.op("dve", lambda e: e.tensor_tensor(out=pr[:, 0, :], in0=gt[:, lo:lo + 64], in1=gt[:, lo + 64:lo + 128], op=ALU.mult), reads=[P.ConstB], writes=[PRB])
    sc.op("dve", lambda e: e.tensor_tensor(out=pr[:, 1, :], in0=gt[:, lo + 128:lo + 192], in1=gt[:, lo + 192:lo + 256], op=ALU.mult), reads=[P.ConstB], writes=[PRB])
    sc.op("dve", lambda e: e.tensor_reduce(out=sm[:, 0:2], in_=pr[:, :, :], axis=AX.X, op=ALU.add), reads=[PRB], writes=[SMB])
    sc.op("act", lambda e: e.activation(out=sm[:, 2:4], in_=sm[:, 0:2], func=AF.Exp), reads=[SMB], writes=[SMB])
    sc.op("dve", lambda e: e.tensor_tensor(out=sm[:, 4:5], in0=sm[:, 2:3], in1=sm[:, 3:4], op=ALU.subtract), reads=[SMB], writes=[SMB])
    sc.op("dve", lambda e: e.tensor_scalar(out=sm[:, 5:6], in0=sm[:, 4:5], scalar1=lam_init, scalar2=-1.0, op0=ALU.add, op1=ALU.mult), reads=[SMB], writes=[SMB])
    dn = VT["diff_norm"]
    sc.op("dve", lambda e: e.tensor_scalar(out=sm[:, 6:7], in0=gt[:, dn:dn + 1], scalar1=1.0 - lam_init, scalar2=None, op0=ALU.mult), reads=[SMB, P.ConstB], writes=[SMB])
    alq_d, alk_d = P.dram["alq"].ap(), P.dram["alk"].ap()
    for h in range(DBG.get('heads', H)):
        sc.dma("pool", [(qa[64:68, 0, :], alq_d[h]), (qa[64:68, 1, :], alq_d[h])], writes=[QRB], key=P.next_wkey())
        sc.dma("pool", [(ka[64:68, 0, :], alk_d[h]), (ka[64:68, 1, :], alk_d[h])], writes=[KRB], key=P.next_wkey())
        for t in range(NTL):
            hv = hall[:, :, t * NT:(t + 1) * NT]
            for isk in range(2):
                for c in range(2):
                    bk = 4 + P.bank() % 4
                    col = isk * 1024 + h * 128 + c * 64

                    def mm(e, bk=bk, col=col, hv=hv):
                        r = None
                        for k in range(NCH):
                            r = e.matmul(P.ps[bk][0:64, :], win[:, k, col:col + 64], hv[:, k, :], start=(k == 0), stop=(k == NCH - 1))
                        return r
                    sc.op("pe", mm, reads=HallB[t] + allW, writes=[P.psB[bk]])
                    if isk == 0:
                        sc.op("act", lambda e, bk=bk, c=c, t=t: e.mul(out=qa[0:64, c, t * NT:(t + 1) * NT], in_=P.ps[bk][0:64, :], mul=0.125),
                              reads=[P.psB[bk]], writes=[QB[t]])
                    else:
                        sc.op("dve", lambda e, bk=bk, c=c, t=t: e.tensor_copy(out=ka[0:64, c, t * NT:(t + 1) * NT], in_=P.ps[bk][0:64, :]),
                              reads=[P.psB[bk]], writes=[KB[t]])
            bk = 4 + P.bank() % 4

            def mmv(e, bk=bk, t=t, h=h):
                r = None
                for tb in range(4):
                    for k in range(NCH):
                        r = e.matmul(P.ps[bk][:, tb * 128:(tb + 1) * 128], hall[:, k, t * NT + tb * 128:t * NT + (tb + 1) * 128],
                                     win[:, k, 2048 + h * 128:2048 + (h + 1) * 128], start=(k == 0), stop=(k == NCH - 1))
                return r
            sc.op("pe", mmv, reads=HallB[t] + allW, writes=[P.psB[bk]])
            sc.op("act", lambda e, bk=bk, t=t: e.activation(out=v[:, 4 * t:4 * t + 4, :], in_=P.ps[bk][:, :].rearrange("p (a b) -> p a b", a=4), func=AF.Copy),
                  reads=[P.psB[bk]], writes=[VB[t]])
        for qt in range(DBG.get('qtiles', NTL)):
            nk = 4 * (qt + 1)
            pend = None
            pcount = 0
            for kc in range(nk + 1):
                cur = None
                if kc < nk:
                    cur = []
                    for c in range(2):
                        bk = 4 + (pcount % 4)
                        pb = pcount % 4
                        pcount += 1
                        j = kc - 4 * qt

                        def mms(e, bk=bk, c=c, kc=kc, j=j, qt=qt):
                            KR = DBG.get('kr', 68)
                            r = e.matmul(P.ps[bk][:, :], ka[0:KR, c, kc * 128:(kc + 1) * 128], qa[0:KR, c, qt * NT:(qt + 1) * NT], start=True, stop=(j < 0))
                            if j >= 0:
                                r = e.matmul(P.ps[bk][:, :], ident[:, :], maskd[:, j, :], start=False, stop=True)
                            return r
                        sc.op("pe", mms, reads=[KB[kc // 4], KRB, QB[qt], QRB, CMB], writes=[P.psB[bk]])
                        sc.op("act", lambda e, bk=bk, pb=pb: e.activation(out=pT[pb][:, :], in_=P.ps[bk][:, :], func=AF.Exp),
                              reads=[P.psB[bk]], writes=[PTB[pb]])
                        cur.append((c, pb, kc))
                if pend is not None:
                    for (c, pb, kk) in pend:
                        def mmp(e, c=c, pb=pb, kk=kk, nk=nk):
                            e.matmul(P.ps[c][:, :], v[:, kk, :], pT[pb][:, :], start=(kk == 0), stop=(kk == nk - 1))
                            return e.matmul(P.ps[2 + c][:, :], P.ones_bf[:, :], pT[pb][:, :], start=(kk == 0), stop=(kk == nk - 1))
                        sc.op("pe", mmp, reads=[VB[kk // 4], PTB[pb]], writes=[P.psB[c], P.psB[2 + c]])
                pend = cur
            for c in range(2):
                sc.op("act", lambda e, c=c: e.activation(out=rd[c][:, :], in_=P.ps[2 + c][:, :], func=AF.Ln), reads=[P.psB[2 + c]], writes=[RDB[c]])
                sc.op("act", lambda e, c=c: e.activation(out=rd[c][:, :], in_=rd[c][:, :], func=AF.Exp, scale=-1.0), reads=[RDB[c]], writes=[RDB[c]])
                sc.op("dve", lambda e, c=c: e.tensor_tensor(out=tt[c][:, :], in0=P.ps[c][:, :], in1=rd[c][:, :], op=ALU.mult),
                      reads=[P.psB[c], RDB[c]], writes=[TTB[c]])
            sc.op("dve", lambda e: e.scalar_tensor_tensor(out=osb[:, :], in0=tt[1][:, :], scalar=sm[:, 5:6], in1=tt[0][:, :], op0=ALU.mult, op1=ALU.add),
                  reads=[TTB[0], TTB[1], SMB], writes=[OSB])
            sc.op("act", lambda e: e.activation(out=sqo[:, :], in_=osb[:, :], func=AF.Square), reads=[OSB], writes=[SQOB])
            bk = 4 + (pcount % 4)
            pcount += 1
            sc.op("pe", lambda e, bk=bk: e.matmul(P.ps[bk][:, :], P.ones_128[:, :], sqo[:, :], start=True, stop=True), reads=[SQOB], writes=[P.psB[bk]])
            sc.op("act", lambda e, bk=bk: e.activation(out=lnr[:, 0, :], in_=P.ps[bk][:, :], func=AF.Ln, bias=1e-5, scale=1.0), reads=[P.psB[bk]], writes=[LNB[0]])
            sc.op("act", lambda e: e.activation(out=lnr[:, 1, :], in_=lnr[:, 0, :], func=AF.Exp, scale=-0.5), reads=[LNB[0]], writes=[LNB[1]])
            s2 = (h * NTL + qt) % 2
            sc.op("dve", lambda e, s2=s2: e.scalar_tensor_tensor(out=mixh[s2][:, :], in0=osb[:, :], scalar=sm[:, 6:7], in1=lnr[:, 1, :], op0=ALU.mult, op1=ALU.mult),
                  reads=[OSB, LNB[1], SMB], writes=[MXB[s2]])
            sc.dma("sp", [(cat_r[:, h, qt * NT:(qt + 1) * NT], mixh[s2][:, :])], reads=[MXB[s2]], writes=[CatMixB[qt]], key=f"mixst{s2}")


def mlstm_mix(P, li, hall, HallB, w_in_d, cat_r, CatMixB):
    lin_mix(P, li, hall, HallB, w_in_d, cat_r, CatMixB, True)


def gla_mix(P, li, hall, HallB, w_in_d, cat_r, CatMixB):
    lin_mix(P, li, hall, HallB, w_in_d, cat_r, CatMixB, False)


def lin_mix(P, li, hall, HallB, w_in_d, cat_r, CatMixB, ML):
    sc = P.sc
    gt = P.vtab
    H = 4
    DKC = 2 if ML else 1
    DVA = 3 if ML else 2
    DV = 256
    DK = 128 * DKC
    NB = S_ // 128
    qscale = float(DK) ** -0.5
    if ML:
        qoff, koff, voff, goff = 0, 1024, 2048, 3072
        ncols = 1024
    else:
        qoff, koff, voff, goff = 0, 512, 1024, 2048
        ncols = 768
    ident = P.alloc("ident", [128, 128], BF16)
    segm = P.alloc("segm", [128, NT], F32)
    cmask = P.alloc("cmask", [128, 64], F32)
    CMB, CM2 = Buf("lconst"), Buf("lconst2")
    sc.dma("pool", [(ident[:, :], P.dram["ident"].ap())], writes=[CMB], key="cstp")
    sc.dma("sp", [(segm[:, :], P.dram["segm"].ap()), (cmask[:, :], P.dram["cmask"].ap())], writes=[CM2], key="cst")
    negb = P.alloc("negb", [128, 8], F32)
    NGB = Buf("negb")
    gb0 = VT["mlstm_gate_b"] if ML else VT["gla_gate_b"]
    sc.op("dve", lambda e: e.tensor_scalar(out=negb[:, 0:8], in0=gt[:, gb0:gb0 + 8], scalar1=-1.0, scalar2=None, op0=ALU.mult),
          reads=[P.ConstB], writes=[NGB])
    win = P.alloc("win", [128, NCH, ncols], BF16)
    if ML:
        wgs = P.alloc("wgs", [128, NCH, 8], BF16)
        wrep = P.alloc("wrep", [128, NCH, 2, 128], BF16)
        WGSB, WREPB = Buf("wgs"), Buf("wrep")
        sc.dma("pool", [(wgs[:, :, :], w_in_d[:, :, 4096:4104])], writes=[WGSB], key=P.next_wkey())
    else:
        wz = P.alloc("wz", [128, NCH, 16], BF16)
        w2 = P.alloc("w2", [16, 512], BF16)
        WZB = Buf("wz")
        sc.dma("pool", [(wz[:, :, :], w_in_d[:, :, 3072:3088]), (w2[:, :], P.dram["gla_gate_w2"].ap()[0])], writes=[WZB], key=P.next_wkey())
    qsL = [P.alloc("qs", [128, DKC, NT], BF16) for _ in range(2)]
    ksL = [P.alloc("ks", [128, DKC, NT], BF16) for _ in range(2)]
    kd = P.alloc("kd", [128, DKC, NT], BF16)
    vtmL = [P.alloc("vtm", [128, 4, DVA * 128], BF16) for _ in range(2)]
    kdtmL = [P.alloc("kdtm", [128, 4, DK], BF16) for _ in range(2)]
    ebL = P.alloc("ebL", [128, 64], F32)
    QSB, KSB, VTB, KDTB, EBLB = bl("qs", 2), bl("ks", 2), bl("vtm", 2), bl("kdtm", 2), bl("ebL", NTL)
    KDB = Buf("kd")
    if ML:
        for s_ in range(2):
            sc.op("pool", lambda e, s_=s_: e.memset(vtmL[s_][:, :, 256:384], 1.0), writes=[VTB[s_]])
        pre = [P.alloc("pre", [128, 4, 3 + NT], F32) for _ in range(2)]
        PREB = [bl("pre", 4) for _ in range(2)]
        cacc = P.alloc("cacc", [128, NT], F32)
        CACB = Buf("cacc")
    qk = P.alloc("qk", [128, 2 * DKC, NT], F32)
    QKB = bl("qk", 2 * DKC)
    g1 = P.alloc("g1", [128, NT], F32)
    g2 = P.alloc("g2", [128, NT], F32)
    g3 = P.alloc("g3", [128, NT], F32)
    g4 = P.alloc("g4", [128, NT], F32)
    g5 = P.alloc("g5", [128, NT], F32)
    g6 = P.alloc("g6", [128, NT], F32)
    G1B, G2B, G3B, G4B, G5B, G6B = (Buf("g1"), Buf("g2"), Buf("g3"), Buf("g4"), Buf("g5"), Buf("g6"))
    zt = P.alloc("zt", [16, NT], BF16)
    ZTB = Buf("zt")
    Sst = P.alloc("Sst", [128, DKC, DVA * 128], F32)
    SbfL = [P.alloc("Sbf", [128, DKC, DVA * 128], BF16) for _ in range(2)]
    SSB = bl("Sst", DKC)
    SBBL = [bl("Sbf", DKC) for _ in range(2)]
    amT = [P.alloc("amT", [128, 128], BF16) for _ in range(2)]
    AMB = bl("amT", 2)
    obuf = [P.alloc("obuf", [128, DVA, NT], F32) for _ in range(2)]
    OBB = [bl("obuf", DVA) for _ in range(2)]
    hn = P.alloc("hn", [128, 2, NT], F32)
    HNB = bl("hn", 2)
    sqn = P.alloc("sqn", [128, 2, NT], BF16)
    SQNB = bl("sqn", 2)
    tmpn = P.alloc("tmpn", [128, 2, NT], F32)
    TNB = bl("tmpn", 2)
    gate = [P.alloc("gate", [128, NT], F32) for _ in range(2)]
    GTB = bl("gate", 2)
    rdn = P.alloc("rdn", [128, NT], F32)
    RDNB = Buf("rdn")
    mixo = [P.alloc("mixo", [128, 2, NT], BF16) for _ in range(2)]
    MXB = [bl("mixo", 2) for _ in range(2)]
    ncol0 = VT["mlstm_norm"] if ML else VT["gla_norm"]
    WB = Buf("winh")
    pbk = [0]

    def pbank():
        pbk[0] = (pbk[0] + 1) % 2
        return 5 + pbk[0]

    for h in range(DBG.get("lheads", H)):
        if ML:
            srcs = [(0, qoff + h * 256, 256), (256, koff + h * 256, 256), (512, voff + h * 256, 256), (768, goff + h * 256, 256)]
        else:
            srcs = [(0, qoff + h * 128, 128), (128, koff + h * 128, 128), (256, voff + h * 256, 256), (512, goff + h * 256, 256)]
        sc.dma("pool", [(win[:, :, d0:d0 + n], w_in_d[:, :, s0:s0 + n]) for (d0, s0, n) in srcs], writes=[WB], key=P.next_wkey())
        wq0, wk0, wv0, wg0 = [s[0] for s in srcs]
        if ML:
            for g in range(2):
                col = g * 4 + h
                sc.op("dve", lambda e, g=g, col=col: e.tensor_copy(out=wrep[:, :, g, :], in_=wgs[:, :, col:col + 1].to_broadcast([128, NCH, 128])),
                      reads=[WGSB], writes=[WREPB])
        for dkc in range(DKC):
            sc.op("dve", lambda e, dkc=dkc: e.memset(Sst[:, dkc, :], 0.0), writes=[SSB[dkc]])
            sc.op("pool", lambda e, dkc=dkc: e.memset(SbfL[0][:, dkc, :], 0.0), writes=[SBBL[0][dkc]])
        def passA(t, h=h):
            hv = hall[:, :, t * NT:(t + 1) * NT]
            HB = HallB[t]
            s = t % 2
            for isk in range(2):
                for dkc in range(DKC):
                    bk = pbank()
                    col = (wk0 if isk else wq0) + dkc * 128
                    idx = isk * DKC + dkc

                    def mm(e, bk=bk, col=col, hv=hv):
                        r = None
                        for k in range(NCH):
                            r = e.matmul(P.ps[bk][:, :], win[:, k, col:col + 128], hv[:, k, :], start=(k == 0), stop=(k == NCH - 1))
                        return r
                    sc.op("pe", mm, reads=HB + [WB], writes=[P.psB[bk]])
                    if ML:
                        pr_ = pre[s]
                        if t == 0:
                            sc.op("dve", lambda e, idx=idx, pr_=pr_: e.memset(pr_[:, idx, 0:3], 0.0), writes=[PREB[s][idx]])
                        else:
                            sc.op("dve", lambda e, idx=idx, pr_=pr_, po=pre[1 - s]: e.tensor_copy(out=pr_[:, idx, 0:3], in_=po[:, idx, NT:NT + 3]),
                                  reads=[PREB[1 - s][idx]], writes=[PREB[s][idx]])
                        sc.op("act", lambda e, idx=idx, pr_=pr_, bk=bk: e.activation(out=pr_[:, idx, 3:3 + NT], in_=P.ps[bk][:, :], func=AF.Copy),
                              reads=[P.psB[bk]], writes=[PREB[s][idx]])
                        c16 = (8 * isk) + 2 * h + dkc
                        cw = VT["conv_w"]
                        sc.op("dve", lambda e, idx=idx, pr_=pr_, c16=c16, cw=cw: e.tensor_scalar(out=cacc[:, :], in0=pr_[:, idx, 3:3 + NT], scalar1=gt[:, cw + 48 + c16:cw + 49 + c16],
                                                                                          scalar2=None, op0=ALU.mult),
                              reads=[PREB[s][idx], P.ConstB], writes=[CACB])
                        for j in range(3):
                            sc.op("dve", lambda e, idx=idx, pr_=pr_, c16=c16, cw=cw, j=j: e.scalar_tensor_tensor(
                                out=cacc[:, :], in0=pr_[:, idx, j:j + NT], scalar=gt[:, cw + j * 16 + c16:cw + j * 16 + c16 + 1], in1=cacc[:, :], op0=ALU.mult, op1=ALU.add),
                                reads=[PREB[s][idx], CACB], writes=[CACB])
                        sc.op("act", lambda e, idx=idx: e.activation(out=qk[:, idx, :], in_=cacc[:, :], func=AF.Silu), reads=[CACB], writes=[QKB[idx]])
                    else:
                        sc.op("act", lambda e, idx=idx, bk=bk: e.activation(out=qk[:, idx, :], in_=P.ps[bk][:, :], func=AF.Copy), reads=[P.psB[bk]], writes=[QKB[idx]])
                    yield
            if ML:
                bi, bf_ = pbank(), pbank()
                for g, bk in ((0, bi), (1, bf_)):
                    def mm(e, g=g, bk=bk, hv=hv):
                        r = None
                        for k in range(NCH):
                            r = e.matmul(P.ps[bk][:, :], wrep[:, k, g, :], hv[:, k, :], start=(k == 0), stop=(k == NCH - 1))
                        return r
                    sc.op("pe", mm, reads=HB + [WREPB], writes=[P.psB[bk]])
                sc.op("act", lambda e, bk=bf_, h=h: e.activation(out=g1[:, :], in_=P.ps[bk][:, :], func=AF.Exp, scale=-1.0, bias=negb[:, 4 + h:5 + h]),
                      reads=[P.psB[bf_], NGB], writes=[G1B])
            else:
                bz = pbank()

                def mm(e, bk=bz, hv=hv):
                    r = None
                    for k in range(NCH):
                        r = e.matmul(P.ps[bk][0:16, :], wz[:, k, :], hv[:, k, :], start=(k == 0), stop=(k == NCH - 1))
                    return r
                sc.op("pe", mm, reads=HB + [WZB], writes=[P.psB[bz]])
                sc.op("act", lambda e, bk=bz: e.activation(out=zt[:, :], in_=P.ps[bk][0:16, :], func=AF.Copy), reads=[P.psB[bz]], writes=[ZTB])
                bx = pbank()
                sc.op("pe", lambda e, bk=bx, h=h: e.matmul(P.ps[bk][:, :], w2[0:16, h * 128:(h + 1) * 128], zt[0:16, :], start=True, stop=True),
                      reads=[ZTB, WZB], writes=[P.psB[bx]])
                sc.op("act", lambda e, bk=bx, h=h: e.activation(out=g1[:, :], in_=P.ps[bk][:, :], func=AF.Exp, scale=-1.0, bias=negb[:, h:h + 1]),
                      reads=[P.psB[bx], NGB], writes=[G1B])
            sc.op("act", lambda e: e.activation(out=g1[:, :], in_=g1[:, :], func=AF.Ln, bias=1.0, scale=1.0), reads=[G1B], writes=[G1B])
            sc.op("dve", lambda e: e.tensor_tensor_scan(out=g2[:, :], data0=segm[:, :], data1=g1[:, :], initial=0.0, op0=ALU.mult, op1=ALU.add),
                  reads=[G1B, CM2], writes=[G2B])
            sc.op("dve", lambda e: e.tensor_scalar(out=g2[:, :], in0=g2[:, :], scalar1=(-1.0 if ML else -1.0 / 16.0), scalar2=None, op0=ALU.mult),
                  reads=[G2B], writes=[G2B])
            sc.op("act", lambda e: e.activation(out=g3[:, :], in_=g2[:, :], func=AF.Exp), reads=[G2B], writes=[G3B])
            if ML:
                sc.op("dve", lambda e, bk=bi, h=h: e.scalar_tensor_tensor(out=g4[:, :], in0=P.ps[bk][:, :], scalar=gt[:, gb0 + h:gb0 + h + 1], in1=g2[:, :],
                                                                   op0=ALU.add, op1=ALU.subtract), reads=[P.psB[bi], G2B, P.ConstB], writes=[G4B])
                sc.op("act", lambda e: e.activation(out=g5[:, :], in_=g4[:, :], func=AF.Exp), reads=[G4B], writes=[G5B])
                for ch in range(8):
                    lc = ch * 64 + 63
                    sc.op("act", lambda e, ch=ch, lc=lc: e.activation(out=g6[:, ch * 64:(ch + 1) * 64], in_=g4[:, ch * 64:(ch + 1) * 64], func=AF.Exp,
                                                                    bias=g2[:, lc:lc + 1], scale=1.0), reads=[G4B, G2B], writes=[G6B])
            else:
                sc.op("act", lambda e: e.activation(out=g5[:, :], in_=g2[:, :], func=AF.Exp, scale=-1.0), reads=[G2B], writes=[G5B])
                for ch in range(8):
                    lc = ch * 64 + 63
                    sc.op("act", lambda e, ch=ch, lc=lc: e.activation(out=g6[:, ch * 64:(ch + 1) * 64], in_=g2[:, ch * 64:(ch + 1) * 64], func=AF.Exp,
                                                                    bias=g2[:, lc:lc + 1], scale=-1.0), reads=[G2B], writes=[G6B])
            yield
            sc.op("act", lambda e, t=t: e.activation(out=ebL[:, t * 8:(t + 1) * 8], in_=g2[:, :].rearrange("p (c l) -> p c l", l=64)[:, :, 63], func=AF.Exp),
                  reads=[G2B], writes=[EBLB[t]])
            yield
            for dkc in range(DKC):
                sc.op("dve", lambda e, dkc=dkc, t=t: e.scalar_tensor_tensor(out=qsL[t % 2][:, dkc, :], in0=qk[:, dkc, :], scalar=qscale, in1=g3[:, :],
                                                                         op0=ALU.mult, op1=ALU.mult), reads=[QKB[dkc], G3B], writes=[QSB[t % 2]])
                sc.op("dve", lambda e, dkc=dkc, t=t: e.tensor_tensor(out=ksL[t % 2][:, dkc, :], in0=qk[:, DKC + dkc, :], in1=g5[:, :], op=ALU.mult),
                      reads=[QKB[DKC + dkc], G5B], writes=[KSB[t % 2]])
                sc.op("dve", lambda e, dkc=dkc: e.tensor_tensor(out=kd[:, dkc, :], in0=qk[:, DKC + dkc, :], in1=g6[:, :], op=ALU.mult),
                      reads=[QKB[DKC + dkc], G6B], writes=[KDB])
            yield
            for tb in range(4):
                bk = pbank()

                def mm(e, bk=bk, tb=tb):
                    r = None
                    for dkc in range(DKC):
                        r = e.matmul(P.ps[bk][:, dkc * 128:(dkc + 1) * 128], kd[:, dkc, tb * 128:(tb + 1) * 128], ident[:, :], start=True, stop=True)
                    return r
                sc.op("pe", mm, reads=[KDB, CMB], writes=[P.psB[bk]])
                sc.op("act", lambda e, bk=bk, tb=tb, t=t: e.activation(out=kdtmL[t % 2][:, tb, :], in_=P.ps[bk][:, 0:DK], func=AF.Copy),
                      reads=[P.psB[bk]], writes=[KDTB[t % 2]])
                bk = pbank()

                def mm(e, bk=bk, tb=tb, t=t):
                    r = None
                    for k in range(NCH):
                        r = e.matmul(P.ps[bk][:, 0:DV], hall[:, k, t * NT + tb * 128:t * NT + (tb + 1) * 128], win[:, k, wv0:wv0 + DV], start=(k == 0), stop=(k == NCH - 1))
                    return r
                sc.op("pe", mm, reads=HB + [WB], writes=[P.psB[bk]])
                sc.op("dve", lambda e, bk=bk, tb=tb, t=t: e.tensor_copy(out=vtmL[t % 2][:, tb, 0:DV], in_=P.ps[bk][:, 0:DV]), reads=[P.psB[bk]], writes=[VTB[t % 2]])
                yield
        def passBC(t, h=h):
            for dc in range(4 * t, 4 * t + 4):
                s = t % 2
                qs, ks, vtm, kdtm = qsL[s], ksL[s], vtmL[s], kdtmL[s]
                cols = slice(dc * 128, (dc + 1) * 128)
                a = dc % 2
                po = 3 + (dc % 2)

                def mms(e, dc=dc, ks=ks, qs=qs):
                    r = None
                    for dkc in range(DKC):
                        r = e.matmul(P.ps[2][:, 0:128], ks[:, dkc, (dc % 4) * 128:(dc % 4 + 1) * 128], qs[:, dkc, (dc % 4) * 128:(dc % 4 + 1) * 128], start=(dkc == 0), stop=(dkc == DKC - 1))
                    return r
                sc.op("pe", mms, reads=[KSB[s], QSB[s]], writes=[P.psB[2]])
                for half in range(2):
                    r0 = half * 64
                    sc.op("dve", lambda e, a=a, r0=r0: e.tensor_tensor(out=amT[a][r0:r0 + 64, r0:r0 + 64], in0=P.ps[2][r0:r0 + 64, r0:r0 + 64], in1=cmask[r0:r0 + 64, :], op=ALU.mult),
                          reads=[P.psB[2], CM2], writes=[AMB[a]])
                for half in range(2):
                    r0 = half * 64
                    ch = dc * 2 + half
                    c0 = (dc % 4) * 128 + r0
                    for dkc in range(DKC):
                        sc.op("pe", lambda e, dkc=dkc, r0=r0, dc=dc, kdtm=kdtm, vtm=vtm: e.matmul(P.ps[dkc][:, 0:DVA * 128], kdtm[r0:r0 + 64, dc % 4, dkc * 128:(dkc + 1) * 128], vtm[r0:r0 + 64, dc % 4, :], start=True, stop=True),
                              reads=[KDTB[s], VTB[s]], writes=[P.psB[dkc]])

                    def mmo(e, a=a, r0=r0, dc=dc, c0=c0, po=po, vtm=vtm, qs=qs, Sbf=SbfL[ch % 2]):
                        r = None
                        for dva in range(DVA):
                            e.matmul(P.ps[po][:, dva * 128 + r0:dva * 128 + r0 + 64], vtm[r0:r0 + 64, dc % 4, dva * 128:(dva + 1) * 128], amT[a][r0:r0 + 64, r0:r0 + 64], start=True, stop=False)
                            for dkc in range(DKC):
                                r = e.matmul(P.ps[po][:, dva * 128 + r0:dva * 128 + r0 + 64], Sbf[:, dkc, dva * 128:(dva + 1) * 128], qs[:, dkc, c0:c0 + 64], start=False, stop=(dkc == DKC - 1))
                        return r
                    sc.op("pe", mmo, reads=[VTB[s], AMB[a], QSB[s]] + SBBL[ch % 2], writes=[P.psB[po]])
                    for dkc in range(DKC):
                        ecol = ch
                        sc.op("dve", lambda e, dkc=dkc, ecol=ecol: e.scalar_tensor_tensor(out=Sst[:, dkc, :], in0=Sst[:, dkc, :], scalar=ebL[:, ecol:ecol + 1], in1=P.ps[dkc][:, 0:DVA * 128],
                                                                                     op0=ALU.mult, op1=ALU.add), reads=[SSB[dkc], EBLB[t], P.psB[dkc]], writes=[SSB[dkc]])
                        if ML:
                            sc.op("act", lambda e, dkc=dkc, Sn=SbfL[(ch + 1) % 2]: e.activation(out=Sn[:, dkc, :], in_=Sst[:, dkc, :], func=AF.Copy), reads=[SSB[dkc]], writes=[SBBL[(ch + 1) % 2][dkc]])
                        else:
                            sc.op("dve", lambda e, dkc=dkc, Sn=SbfL[(ch + 1) % 2]: e.tensor_copy(out=Sn[:, dkc, :], in_=Sst[:, dkc, :]), reads=[SSB[dkc]], writes=[SBBL[(ch + 1) % 2][dkc]])
                    yield
                tb = dc % 4
                for dva in range(DVA):
                    sc.op("act", lambda e, dva=dva, tb=tb, s=s, po=po: e.activation(out=obuf[s][:, dva, tb * 128:(tb + 1) * 128], in_=P.ps[po][:, dva * 128:(dva + 1) * 128], func=AF.Copy),
                          reads=[P.psB[po]], writes=[OBB[s][dva]])
                if tb != 3:
                    continue
                ob = obuf[s]
                if ML:
                    sc.op("dve", lambda e, ob=ob: e.scalar_tensor_tensor(out=rdn[:, :], in0=ob[:, 2, :], scalar=-1.0, in1=ob[:, 2, :], op0=ALU.mult, op1=ALU.max), reads=[OBB[s][2]], writes=[RDNB])
                    sc.op("dve", lambda e: e.tensor_scalar(out=rdn[:, :], in0=rdn[:, :], scalar1=1.0, scalar2=None, op0=ALU.max), reads=[RDNB], writes=[RDNB])
                    sc.op("act", lambda e: e.activation(out=rdn[:, :], in_=rdn[:, :], func=AF.Ln), reads=[RDNB], writes=[RDNB])
                    sc.op("act", lambda e: e.activation(out=rdn[:, :], in_=rdn[:, :], func=AF.Exp, scale=-1.0), reads=[RDNB], writes=[RDNB])
                    for dvc in range(2):
                        sc.op("dve", lambda e, ob=ob, dvc=dvc: e.tensor_tensor(out=ob[:, dvc, :], in0=ob[:, dvc, :], in1=rdn[:, :], op=ALU.mult),
                              reads=[OBB[s][dvc], RDNB], writes=[OBB[s][dvc]])
                nb0 = ncol0 + h * 2
                rmsnorm_tile(P, ob, OBB[s], lambda c: gt[:, nb0 + c:nb0 + c + 1], hn, HNB, sqn, SQNB, tmpn, TNB, nch=2, ones=P.ones_256)
                yield
                hv = hall[:, :, t * NT:(t + 1) * NT]
                for dvc in range(2):
                    bk = pbank()
                    col = wg0 + dvc * 128

                    def mm(e, bk=bk, col=col, hv=hv):
                        r = None
                        for k in range(NCH):
                            r = e.matmul(P.ps[bk][:, :], win[:, k, col:col + 128], hv[:, k, :], start=(k == 0), stop=(k == NCH - 1))
                        return r
                    sc.op("pe", mm, reads=HallB[t] + [WB], writes=[P.psB[bk]])
                    sc.op("act", lambda e, bk=bk, dvc=dvc: e.activation(out=gate[dvc][:, :], in_=P.ps[bk][:, :], func=(AF.Sigmoid if ML else AF.Silu)),
                          reads=[P.psB[bk]], writes=[GTB[dvc]])
                    sc.op("dve", lambda e, dvc=dvc, s=s: e.tensor_tensor(out=mixo[s][:, dvc, :], in0=hn[:, dvc, :], in1=gate[dvc][:, :], op=ALU.mult),
                          reads=[HNB[dvc], GTB[dvc]], writes=[MXB[s][dvc]])
                sc.dma("sp", [(cat_r[:, 2 * h:2 * h + 2, t * NT:(t + 1) * NT], mixo[s][:, :, :])], reads=MXB[s], writes=[CatMixB[t]], key=f"mixst{s}")


        for _ in passA(0):
            pass
        for t in range(NTL):
            ga = passA(t + 1) if t + 1 < NTL else iter(())
            for _ in passBC(t):
                for _k in range(DBG.get("astep", 2)):
                    next(ga, None)
            for _ in ga:
                pass


def _bank(self):
    self._bk = (getattr(self, "_bk", -1) + 1) % 7
    return self._bk


Prog.bank = _bank


def build(nstages=99):
    nc = bass.Bass("TRN2", target_bir_lowering=False)
    P = Prog(nc)
    sc = P.sc
    xT = P.din("xT", [D_, S_])
    memT = P.din("memT", [D_, MEMLEN])
    for k, shp in WEIGHT_SHAPES.items():
        P.din(k, shp)
    vtab_d = P.din("vtab", [128, NV])
    inv0_d = P.din("inv0", [128, 4, NT])
    P.din("maskd", [128, 4, NT])
    P.din("ident", [128, 128])
    P.din("alq", [8, 4, S_])
    P.din("alk", [8, 4, S_])
    P.din("segm", [128, NT])
    P.din("cmask", [128, 64])
    outT = P.dout("outT", [D_, S_])
    xr = P.dint("xr", [D_, S_])
    P.dint("catd", [1536, S_], BF16)
    P.vtab = P.const("vtab_s", [128, NV], F32)
    P.gtab = P.vtab
    P.ones_mean = P.const("ones_mean", [128, 128], BF16)
    P.ones_bf = P.const("ones_bf", [128, 128], BF16)
    P.ones_128 = P.const("ones_128", [128, 128], BF16)
    P.ones_256 = P.const("ones_256", [128, 128], BF16)
    P.mem_n = P.const("mem_n", [128, NCH, MEMLEN], BF16)
    P.ConstB = Buf("consts")
    P.MemNB = Buf("mem_n")
    CB = P.ConstB
    sc.dma("sp", [(P.vtab[:, :], vtab_d.ap())], writes=[CB], key="cst")
    OB = Buf("ones")
    sc.op("dve", lambda e: e.memset(P.ones_mean[:, :], 1.0 / 1024.0), writes=[OB])
    sc.op("dve", lambda e: e.memset(P.ones_bf[:, :], 1.0), writes=[OB])
    sc.op("dve", lambda e: e.memset(P.ones_128[:, :], 1.0 / 128.0), writes=[OB])
    sc.op("dve", lambda e: e.memset(P.ones_256[:, :], 1.0 / 256.0), writes=[OB])
    sc.barrier()
    P.epsc = lambda eps: float(eps)
    P.arena_start()
    P.stage_begin()
    mt = P.alloc("memT", [128, NCH, MEMLEN], F32)
    msq = P.alloc("msq", [128, NCH, MEMLEN], BF16)
    mtmp = P.alloc("mtmp", [128, 2, MEMLEN], F32)
    MTB, MSQB, MTMB = bl("mt", NCH), bl("msq", NCH), bl("mtmp", 2)
    sc.dma("sp", [(mt[:, :, :], memT.ap().rearrange("(c p) n -> p c n", p=128))], writes=MTB, key="xld0")
    mg = VT["norm"] + GI_MEM * NCH
    MNB = bl("memn", NCH)
    rmsnorm_tile(P, mt, MTB, lambda c: P.vtab[:, mg + c:mg + c + 1], P.mem_n, MNB, msq, MSQB, mtmp, MTMB, n=MEMLEN)
    sc.op("dve", lambda e: e.memset(mtmp[:, 0, 0:1], 0.0), reads=MNB, writes=[P.MemNB])
    XinB = bl("xTd", NTL)
    XrB = bl("xrd", NTL)
    OutB = bl("outd", NTL)
    stages = []
    for li in range(DEPTH):
        stages.append(("ffn", li, 0))
        stages.append(("mix", li))
        stages.append(("ffn", li, 1))
    cur, curB = xT, XinB
    sel = stages[:nstages]
    if 'only' in DBG:
        sel = [stages[i] for i in DBG['only']]
    for st in sel:
        if st[0] == "ffn":
            ffn_stage(P, st[1], st[2], cur, xr, curB, XrB)
        else:
            mixer_stage(P, st[1], cur, xr, curB, XrB)
        cur, curB = xr, XrB
    final_stage(P, cur, outT, curB, OutB, norm=(nstages >= len(stages)))
    sc.finish(OutB)
    sc.emit()
    return nc


def host_inputs(inputs, b):
    m = {}
    m["xT"] = np.ascontiguousarray(inputs["x"][b].T)
    m["memT"] = np.ascontiguousarray(inputs["mem"][b].T)
    for k in WEIGHT_SHAPES:
        m[k] = np.ascontiguousarray(inputs[k], dtype=np.float32)
    vt = np.zeros((128, NV), np.float32)

    def cols(v):
        return np.asarray(v, np.float32).reshape(-1, 128).T
    g = np.concatenate([inputs["norm_g"].reshape(12, D_), inputs["final_norm_g"].reshape(1, D_),
                        inputs["mem_norm_g"].reshape(1, D_)], axis=0)
    vt[:, VT["norm"]:VT["norm"] + 14 * NCH] = g.reshape(14, NCH, 128).transpose(2, 0, 1).reshape(128, 14 * NCH)
    vt[:, VT["pool_scale"]:VT["pool_scale"] + 8] = cols(inputs["pool_scale"][0])
    vt[:, VT["diff_norm"]:VT["diff_norm"] + 1] = cols(inputs["diff_norm_g"][0])
    vt[:, VT["mlstm_norm"]:VT["mlstm_norm"] + 8] = cols(inputs["mlstm_norm_g"][0])
    vt[:, VT["gla_norm"]:VT["gla_norm"] + 8] = cols(inputs["gla_norm_g"][0])
    cw = inputs["mlstm_conv_w"][0]
    vt[:, VT["conv_w"]:VT["conv_w"] + 64] = cw.reshape(4, 16, 128).transpose(2, 0, 1).reshape(128, 64)
    vt[:, VT["gla_gate_b"]:VT["gla_gate_b"] + 4] = cols(inputs["gla_gate_b"][0])
    vt[:, VT["mlstm_gate_b"]:VT["mlstm_gate_b"] + 8] = np.broadcast_to(inputs["mlstm_gate_b"][0].reshape(1, 8), (128, 8))
    vt[:, VT["diff_lambda"]:VT["diff_lambda"] + 256] = np.broadcast_to(inputs["diff_lambda"][0].reshape(1, 256), (128, 256))
    m["vtab"] = vt
    m.update(CONSTS)
    return m


def _make_consts():
    c = {}
    inv0 = np.zeros((128, 4, NT), np.float32)
    tpos = np.arange(NT)
    for g, w in enumerate((2, 4, 8, 16)):
        inv0[:, g, :] = (1.0 / np.minimum(tpos + 1, w))[None, :]
    c["inv0"] = inv0
    ki = np.arange(128)[:, None]
    qi = np.arange(NT)[None, :]
    c["maskd"] = np.stack([np.where(qi >= 128 * j + ki, 0.0, -30000.0) for j in range(4)], axis=1).astype(np.float32)
    c["ident"] = np.eye(128, dtype=np.float32)
    pos = np.arange(S_)
    alq = np.zeros((8, 4, S_), np.float32)
    alk = np.zeros((8, 4, S_), np.float32)
    for h in range(8):
        s = 2.0 ** (-(h + 1))
        alq[h] = np.stack([-s * 128.0 * (pos // 128), -s * (pos % 128), np.ones(S_), np.ones(S_)])
        alk[h] = np.stack([np.ones(S_), np.ones(S_), s * 128.0 * (pos // 128), s * (pos % 128)])
    c["alq"], c["alk"] = alq, alk
    c["segm"] = np.broadcast_to((np.arange(NT) % 64 != 0).astype(np.float32)[None, :], (128, NT)).copy()
    sidx = (np.arange(128) % 64)[:, None]
    c["cmask"] = (sidx <= np.arange(64)[None, :]).astype(np.float32)
    return c


CONSTS = _make_consts()


def kernel(**inputs):
    inputs = {k: np.asarray(v) for k, v in inputs.items()}
    nc = build()
    B = inputs["x"].shape[0]
    in_maps = [host_inputs(inputs, b) for b in range(B)]
    res = run_bass_kernel_spmd(nc, in_maps, core_ids=list(range(B)))
    out = np.stack([np.ascontiguousarray(res.results[b]["outT"].T) for b in range(B)], axis=0)
    return out.astype(np.float32)
```

```python
import numpy as np
import concourse.bass as bass
import concourse.mybir as mybir
from concourse.bass_utils import run_bass_kernel_spmd

F32 = mybir.dt.float32
BF16 = mybir.dt.bfloat16
AF = mybir.ActivationFunctionType
ALU = mybir.AluOpType
AX = mybir.AxisListType


class Buf:
    __slots__ = ("name", "lw", "rd")

    def __init__(self, name):
        self.name = name
        self.lw = None
        self.rd = {}


class Sched:
    def __init__(self, nc):
        self.nc = nc
        self.prog = {e: [] for e in ("pe", "act", "dve", "pool", "sp")}
        self.sems = {}
        self.waited = {e: {} for e in self.prog}
        self.ninst = {e: 0 for e in self.prog}

    def _sem(self, key):
        if key not in self.sems:
            self.sems[key] = [self.nc.alloc_semaphore("s_" + key), 0]
        return self.sems[key]

    def _deps(self, eng, reads, writes):
        need = {}

        def add(tok, skip_same):
            if tok is None:
                return
            key, val, src = tok
            if skip_same and src == eng:
                return
            if need.get(key, 0) < val:
                need[key] = val

        for b in reads:
            add(b.lw, eng == "pe")
        for b in writes:
            add(b.lw, True)
            for t in b.rd.values():
                add(t, True)
        waits = []
        w = self.waited[eng]
        for key, val in need.items():
            if w.get(key, 0) < val:
                w[key] = val
                waits.append((self.sems[key][0], val))
        return waits

    def op(self, eng, fn, reads=(), writes=()):
        waits = self._deps(eng, reads, writes)
        s = self._sem(eng)
        s[1] += 1
        tok = (eng, s[1], eng)
        for b in reads:
            b.rd[eng] = tok
        for b in writes:
            b.lw = tok
            b.rd = {}
        sem = s[0]

        def run(e):
            for (h, v) in waits:
                e.wait_ge(h, v)
            fn(e).then_inc(sem, 1)

        self.prog[eng].append(run)
        self.ninst[eng] += 1

    def dma(self, q, pairs, reads=(), writes=(), key=None):
        waits = self._deps(q, reads, writes)
        s = self._sem(key)
        if s[1] > 0 and self.waited[q].get(key, 0) < s[1]:
            self.waited[q][key] = s[1]
            waits.append((s[0], s[1]))
        s[1] += 16 * len(pairs)
        tok = (key, s[1], None)
        for b in reads:
            b.rd[key] = tok
        for b in writes:
            b.lw = tok
            b.rd = {}
        sem = s[0]

        def run(e):
            for (h, v) in waits:
                e.wait_ge(h, v)
            for (o, i) in pairs:
                e.dma_start(out=o, in_=i).then_inc(sem, 16)

        self.prog[q].append(run)
        self.ninst[q] += len(pairs)

    def finish(self, bufs):
        waits = self._deps("sp", bufs, ())

        def run(e):
            for (h, v) in waits:
                e.wait_ge(h, v)

        self.prog["sp"].append(run)

    def emit(self):
        nc = self.nc
        prog = self.prog
        with nc.Block() as block:
            @block.tensor
            def _(e):
                for f in prog["pe"]:
                    f(e)

            @block.scalar
            def _(e):
                for f in prog["act"]:
                    f(e)

            @block.vector
            def _(e):
                for f in prog["dve"]:
                    f(e)

            @block.gpsimd
            def _(e):
                for f in prog["pool"]:
                    f(e)

            @block.sync
            def _(e):
                for f in prog["sp"]:
                    f(e)


S_ = 4096
D_ = 1024
DFF = 2816
NT = 512
NTL = S_ // NT
NCH = D_ // 128
NJ = DFF // 128
DEPTH = 4
MEMLEN = 256


def bl(name, n):
    return [Buf(f"{name}{i}") for i in range(n)]


class Prog:
    def __init__(self, nc):
        self.nc = nc
        self.sc = Sched(nc)
        self.dram = {}
        self.uid = 0
        self.ps = [nc.alloc_psum_tensor(f"psb{i}", [128, 512], F32) for i in range(8)]
        self.psB = bl("psb", 8)
        self.arena_base = None
        self.arena_off = 0
        self.wkey = 0

    def din(self, name, shape, dtype=F32):
        t = self.nc.dram_tensor(name, list(shape), dtype, kind="ExternalInput")
        self.dram[name] = t
        return t

    def dint(self, name, shape, dtype=F32):
        t = self.nc.dram_tensor(name, list(shape), dtype, kind="Internal")
        self.dram[name] = t
        return t

    def dout(self, name, shape, dtype=F32):
        t = self.nc.dram_tensor(name, list(shape), dtype, kind="ExternalOutput")
        self.dram[name] = t
        return t

    def const(self, name, shape, dtype):
        return self.nc.alloc_sbuf_tensor(name, list(shape), dtype)

    def arena_start(self):
        nc = self.nc
        total = nc.SBUF_PARTITION_SIZE_BYTES
        rem = nc.sbuf_bytes_remaining
        self.arena_base = ((total - rem + 63) // 64) * 64
        self.arena_end = total
        self.arena_off = self.arena_base

    def stage_begin(self):
        self.sc.barrier()
        self.arena_off = self.arena_base

    def alloc(self, name, shape, dtype):
        esz = 4 if dtype == F32 else 2
        nbytes = esz
        for s in shape[1:]:
            nbytes *= s
        off = self.arena_off
        self.arena_off = ((off + nbytes + 63) // 64) * 64
        assert self.arena_off <= self.arena_end, (name, self.arena_off, self.arena_end)
        self.uid += 1
        return self.nc.alloc_sbuf_tensor_at(f"{name}_{self.uid}", list(shape), dtype, offset=off)

    def next_wkey(self):
        self.wkey = (self.wkey + 1) % 6
        return f"w{self.wkey}"


def _barrier(self):
    snap = {k: v[1] for k, v in self.sems.items() if v[1] > 0}
    for eng in self.prog:
        waits = []
        w = self.waited[eng]
        for key, val in snap.items():
            if key == eng:
                continue
            if w.get(key, 0) < val:
                w[key] = val
                waits.append((self.sems[key][0], val))
        if waits:
            def run(e, waits=waits):
                for (h, v) in waits:
                    e.wait_ge(h, v)
            self.prog[eng].append(run)


Sched.barrier = _barrier


def rmsnorm_tile(P, X, XB, gcol, h, HB, sq, SQB, tmp, TB, eps=1e-6, nch=NCH, ones=None, n=NT,
                 split_pool=False):
    sc = P.sc
    pm = P.ps[7]
    PMB = P.psB[7]
    ones = P.ones_mean if ones is None else ones
    sc.op("act", lambda e: e.activation(out=sq[:, 0:nch, :n], in_=X[:, 0:nch, :n], func=AF.Square),
          reads=list(XB[:nch]), writes=list(SQB[:nch]))

    def mm(e):
        r = None
        for c in range(nch):
            r = e.matmul(pm[:, :n], ones[:, :], sq[:, c, :n], start=(c == 0), stop=(c == nch - 1))
        return r
    sc.op("pe", mm, reads=SQB[:nch], writes=[PMB])
    sc.op("act", lambda e: e.activation(out=tmp[:, 0, :n], in_=pm[:, :n], func=AF.Ln, bias=P.epsc(eps), scale=1.0),
          reads=[PMB], writes=[TB[0]])
    sc.op("act", lambda e: e.activation(out=tmp[:, 1, :n], in_=tmp[:, 0, :n], func=AF.Exp, scale=-0.5),
          reads=[TB[0]], writes=[TB[1]])
    for c in range(nch):
        eng = "pool" if (split_pool and c % 2 == 1) else "dve"
        gap = gcol(c)
        sc.op(eng, lambda e, c=c, gap=gap: e.scalar_tensor_tensor(out=h[:, c, :n], in0=X[:, c, :n], scalar=gap,
                                                                  in1=tmp[:, 1, :n], op0=ALU.mult, op1=ALU.mult),
              reads=[XB[c], TB[1]], writes=[HB[c]])


def ffn_stage(P, li, fi, xin, xout, XinB, XoutB):
    sc = P.sc
    P.stage_begin()
    wup = P.alloc("wup", [128, NCH, 2 * DFF], BF16)
    wdn = P.alloc("wdn", [128, NJ, D_], BF16)
    Xs = [P.alloc("X", [128, NCH, NT], F32) for _ in range(2)]
    h = P.alloc("h", [128, NCH, NT], BF16)
    act = P.alloc("act", [128, NJ, NT], BF16)
    tmp = P.alloc("tmp", [128, 2, NT], F32)
    sg = [P.alloc("sg", [128, NT], BF16) for _ in range(2)]
    XB = [bl("X", NCH) for _ in range(2)]
    HB = bl("h", NCH)
    AB = bl("act", NJ)
    TB = bl("tmp", 2)
    SGB = bl("sg", 2)
    wu_d = P.dram["ffn_w_up"].ap()[li, fi].rearrange("(c p) f -> p c f", p=128)
    wd_d = P.dram["ffn_w_down"].ap()[li, fi].rearrange("(j p) d -> p j d", p=128)
    WUB = bl("wu", 11)
    WDB = bl("wd", 11)
    for jb in range(11):
        c0 = jb * 256
        sc.dma("pool", [(wup[:, :, c0:c0 + 256], wu_d[:, :, c0:c0 + 256]),
                        (wup[:, :, DFF + c0:DFF + c0 + 256], wu_d[:, :, DFF + c0:DFF + c0 + 256])],
               writes=[WUB[jb]], key=P.next_wkey())
    for jb in range(11):
        sc.dma("pool", [(wdn[:, 2 * jb:2 * jb + 2, :], wd_d[:, 2 * jb:2 * jb + 2, :])],
               writes=[WDB[jb]], key=P.next_wkey())
    xin_r = xin.ap().rearrange("(c p) n -> p c n", p=128)
    xout_r = xout.ap().rearrange("(c p) n -> p c n", p=128)
    gt = P.gtab
    gbase = (li * 3 + (0 if fi == 0 else 2)) * NCH

    def load(t):
        s = t % 2
        sc.dma("sp", [(Xs[s][:, :, :], xin_r[:, :, t * NT:(t + 1) * NT])], reads=[XinB[t]], writes=XB[s],
               key=f"xld{s}")

    load(0)
    for t in range(NTL):
        s = t % 2
        X = Xs[s]
        if t + 1 < NTL:
            load(t + 1)
        rmsnorm_tile(P, X, XB[s], lambda c: gt[:, gbase + c:gbase + c + 1], h, HB, act, AB, tmp, TB)
        for j in range(NJ):
            k = j % 2
            pg, pu = P.ps[2 * k], P.ps[2 * k + 1]

            def mm(e, j=j, pg=pg, pu=pu):
                for c in range(NCH):
                    e.matmul(pg[:, :], wup[:, c, j * 128:(j + 1) * 128], h[:, c, :], start=(c == 0), stop=(c == NCH - 1))
                r = None
                for c in range(NCH):
                    r = e.matmul(pu[:, :], wup[:, c, DFF + j * 128:DFF + (j + 1) * 128], h[:, c, :], start=(c == 0),
                                 stop=(c == NCH - 1))
                return r
            sc.op("pe", mm, reads=HB + [WUB[j // 2]], writes=[P.psB[2 * k], P.psB[2 * k + 1]])
            sc.op("act", lambda e, k=k, pg=pg: e.activation(out=sg[k][:, :], in_=pg[:, :], func=AF.Silu),
                  reads=[P.psB[2 * k]], writes=[SGB[k]])
            sc.op("dve", lambda e, k=k, pu=pu, j=j: e.tensor_tensor(out=act[:, j, :], in0=sg[k][:, :], in1=pu[:, :], op=ALU.mult),
                  reads=[SGB[k], P.psB[2 * k + 1]], writes=[AB[j]])
        for c in range(NCH):
            k = 4 + (c % 2)
            pd = P.ps[k]

            def mm2(e, c=c, pd=pd):
                r = None
                for j in range(NJ):
                    r = e.matmul(pd[:, :], wdn[:, j, c * 128:(c + 1) * 128], act[:, j, :], start=(j == 0), stop=(j == NJ - 1))
                return r
            sc.op("pe", mm2, reads=AB + WDB, writes=[P.psB[k]])
            sc.op("dve", lambda e, c=c, pd=pd, X=X: e.scalar_tensor_tensor(out=X[:, c, :], in0=pd[:, :], scalar=0.5, in1=X[:, c, :],
                                                                      op0=ALU.mult, op1=ALU.add),
                  reads=[P.psB[k], XB[s][c]], writes=[XB[s][c]])
        sc.dma("sp", [(xout_r[:, :, t * NT:(t + 1) * NT], X[:, :, :])], reads=XB[s], writes=[XoutB[t]], key=f"xst{s}")


def final_stage(P, xin, xout, XinB, XoutB, norm=True):
    sc = P.sc
    P.stage_begin()
    Xs = [P.alloc("X", [128, NCH, NT], F32) for _ in range(2)]
    Ys = [P.alloc("Y", [128, NCH, NT], F32) for _ in range(2)]
    sq = P.alloc("sq", [128, NCH, NT], BF16)
    tmp = P.alloc("tmp", [128, 2, NT], F32)
    XB = [bl("X", NCH) for _ in range(2)]
    YB = [bl("Y", NCH) for _ in range(2)]
    SQB = bl("sq", NCH)
    TB = bl("tmp", 2)
    xin_r = xin.ap().rearrange("(c p) n -> p c n", p=128)
    xout_r = xout.ap().rearrange("(c p) n -> p c n", p=128)
    gbase = DEPTH * 3 * NCH
    gt = P.gtab
    for t in range(NTL):
        s = t % 2
        sc.dma("sp", [(Xs[s][:, :, :], xin_r[:, :, t * NT:(t + 1) * NT])], reads=[XinB[t]], writes=XB[s], key=f"xld{s}")
        if norm:
            rmsnorm_tile(P, Xs[s], XB[s], lambda c: gt[:, gbase + c:gbase + c + 1], Ys[s], YB[s], sq, SQB, tmp, TB)
            sc.dma("sp", [(xout_r[:, :, t * NT:(t + 1) * NT], Ys[s][:, :, :])], reads=YB[s], writes=[XoutB[t]], key=f"xst{s}")
        else:
            sc.dma("sp", [(xout_r[:, :, t * NT:(t + 1) * NT], Xs[s][:, :, :])], reads=XB[s], writes=[XoutB[t]], key=f"xst{s}")


WEIGHT_SHAPES = {
    "ffn_w_up": (4, 2, 1024, 5632), "ffn_w_down": (4, 2, 2816, 1024), "mem_w_kv": (4, 1024, 1024),
    "pool_w_in": (1, 1024, 1536), "pool_w_group": (1, 4, 256, 256), "pool_w_out": (1, 1536, 1024),
    "diff_w_in": (1, 1024, 3584), "diff_w_out": (1, 1536, 1024),
    "mlstm_w_in": (1, 1024, 4616), "mlstm_w_out": (1, 1536, 1024),
    "gla_w_in": (1, 1024, 3600), "gla_w_out": (1, 1536, 1024), "gla_gate_w2": (1, 16, 512),
}
VT = {}
_off = 0
for _n, _w in [("norm", 14 * NCH), ("pool_scale", 8), ("diff_norm", 1), ("mlstm_norm", 8), ("gla_norm", 8),
               ("conv_w", 64), ("gla_gate_b", 4), ("mlstm_gate_b", 8), ("diff_lambda", 256)]:
    VT[_n] = _off
    _off += _w
NV = _off
GI_FINAL = 12
GI_MEM = 13
DBG = {}
MIX_NAMES = ["pool", "diff", "mlstm", "gla"]
XQ_OFF = [1024, 3072, 4104, 3088]
IN_W = [1536, 3584, 4616, 3600]


def load_w(P, dst, src, key_bufs, nsplit, axis=2):
    n = dst.shape[axis]
    step = (n + nsplit - 1) // nsplit
    bufs = []
    for i in range(nsplit):
        a, b = i * step, min(n, (i + 1) * step)
        if a >= b:
            break
        B = Buf(f"{key_bufs}{i}")
        if axis == 2:
            P.sc.dma("pool", [(dst[:, :, a:b], src[:, :, a:b])], writes=[B], key=P.next_wkey())
        else:
            P.sc.dma("pool", [(dst[:, a:b, :], src[:, a:b, :])], writes=[B], key=P.next_wkey())
        bufs.append(B)
    return bufs


def mixer_stage(P, li, xin, xout, XinB, XoutB):
    sc = P.sc
    kind = li % 4
    name = MIX_NAMES[kind]
    P.stage_begin()
    gt = P.vtab
    w_in_d = P.dram[name + "_w_in"].ap()[0].rearrange("(c p) f -> p c f", p=128)
    w_out_d = P.dram[name + "_w_out"].ap()[0].rearrange("(k p) d -> p k d", p=128)
    wkv_d = P.dram["mem_w_kv"].ap()[li].rearrange("(c p) f -> p c f", p=128)
    xin_r = xin.ap().rearrange("(c p) n -> p c n", p=128)
    xout_r = xout.ap().rearrange("(c p) n -> p c n", p=128)
    cat_r = P.dram["catd"].ap().rearrange("(k p) n -> p k n", p=128)
    CatMixB, CatXoB = bl("catmix", NTL), bl("catxo", NTL)
    hall = P.alloc("hall", [128, NCH, S_], BF16)
    HallB = [bl("hall", NCH) for _ in range(NTL)]
    mark = P.arena_off
    wkv = P.alloc("wkv", [128, NCH, 1024], BF16)
    wxq = P.alloc("wxq", [128, NCH, 512], BF16)
    memK = P.alloc("memK", [128, 4, MEMLEN], BF16)
    memV = P.alloc("memV", [128, 2, 512], BF16)
    Xs = [P.alloc("X", [128, NCH, NT], F32) for _ in range(2)]
    sq = P.alloc("sq", [128, NCH, NT], BF16)
    tmp = P.alloc("tmp", [128, 2, NT], F32)
    xq = P.alloc("xq", [128, 4, NT], BF16)
    pT = [P.alloc("pT", [128, 2, NT], BF16) for _ in range(2)]
    rden = [P.alloc("rden", [128, NT], F32) for _ in range(2)]
    xo = [P.alloc("xo", [128, 4, NT], BF16) for _ in range(2)]
    XB = [bl("X", NCH) for _ in range(2)]
    SQB, TB = bl("sq", NCH), bl("tmp", 2)
    XQB = bl("xq", 4)
    PTB, RDB = bl("pT", 2), bl("rden", 2)
    XOB = [bl("xo", 4) for _ in range(2)]
    WKVB = load_w(P, wkv, wkv_d, "wkv", 2)
    WXQB = load_w(P, wxq, w_in_d[:, :, XQ_OFF[kind]:XQ_OFF[kind] + 512], "wxq", 1)
    MKB, MVB = Buf("memK"), Buf("memV")
    mem_n = P.mem_n
    for hh in range(4):
        bk = P.bank()

        def mm(e, hh=hh, bk=bk):
            r = None
            for k in range(NCH):
                r = e.matmul(P.ps[bk][:, :MEMLEN], wkv[:, k, hh * 128:(hh + 1) * 128], mem_n[:, k, :], start=(k == 0), stop=(k == NCH - 1))
            return r
        sc.op("pe", mm, reads=[WKVB[0], P.MemNB], writes=[P.psB[bk]])
        sc.op("act", lambda e, hh=hh, bk=bk: e.activation(out=memK[:, hh, :], in_=P.ps[bk][:, :MEMLEN], func=AF.Copy),
              reads=[P.psB[bk]], writes=[MKB])
    for mc in range(2):
        bk = P.bank()

        def mm(e, mc=mc, bk=bk):
            r = None
            for k in range(NCH):
                r = e.matmul(P.ps[bk][:, :], mem_n[:, k, mc * 128:(mc + 1) * 128], wkv[:, k, 512:1024], start=(k == 0), stop=(k == NCH - 1))
            return r
        sc.op("pe", mm, reads=[WKVB[1], P.MemNB], writes=[P.psB[bk]])
        sc.op("act", lambda e, mc=mc, bk=bk: e.activation(out=memV[:, mc, :], in_=P.ps[bk][:, :], func=AF.Copy),
              reads=[P.psB[bk]], writes=[MVB])
    gbase = VT["norm"] + (li * 3 + 1) * NCH

    def loadx(t, Xs=Xs, XB=XB):
        s = t % 2
        sc.dma("sp", [(Xs[s][:, :, :], xin_r[:, :, t * NT:(t + 1) * NT])], reads=[XinB[t]], writes=XB[s], key=f"xld{s}")
    def normx(t):
        rmsnorm_tile(P, Xs[t % 2], XB[t % 2], lambda c: gt[:, gbase + c:gbase + c + 1], hall[:, :, t * NT:(t + 1) * NT], HallB[t], sq, SQB, tmp, TB)
    loadx(0)
    loadx(1)
    normx(0)
    for t in range(NTL):
        s = t % 2
        if t + 1 < NTL:
            normx(t + 1)
        if t + 2 < NTL:
            loadx(t + 2)
        hv = hall[:, :, t * NT:(t + 1) * NT]
        for hh in range(4):
            bk = P.bank()

            def mm(e, hh=hh, bk=bk, hv=hv):
                r = None
                for k in range(NCH):
                    r = e.matmul(P.ps[bk][:, :], wxq[:, k, hh * 128:(hh + 1) * 128], hv[:, k, :], start=(k == 0), stop=(k == NCH - 1))
                return r
            sc.op("pe", mm, reads=HallB[t] + WXQB, writes=[P.psB[bk]])
            sc.op("act", lambda e, hh=hh, bk=bk: e.mul(out=xq[:, hh, :], in_=P.ps[bk][:, :], mul=128.0 ** -0.5),
                  reads=[P.psB[bk]], writes=[XQB[hh]])
        for hh in range(4):
            k2 = hh % 2
            for mc in range(2):
                bk = P.bank()
                sc.op("pe", lambda e, hh=hh, mc=mc, bk=bk: e.matmul(P.ps[bk][:, :], memK[:, hh, mc * 128:(mc + 1) * 128], xq[:, hh, :], start=True, stop=True),
                      reads=[MKB, XQB[hh]], writes=[P.psB[bk]])
                sc.op("act", lambda e, k2=k2, mc=mc, bk=bk: e.activation(out=pT[k2][:, mc, :], in_=P.ps[bk][:, :], func=AF.Exp),
                      reads=[P.psB[bk]], writes=[PTB[k2]])
            bo, bd = P.bank(), P.bank()

            def mm(e, hh=hh, k2=k2, bo=bo, bd=bd):
                e.matmul(P.ps[bo][:, :], memV[:, 0, hh * 128:(hh + 1) * 128], pT[k2][:, 0, :], start=True, stop=False)
                e.matmul(P.ps[bo][:, :], memV[:, 1, hh * 128:(hh + 1) * 128], pT[k2][:, 1, :], start=False, stop=True)
                e.matmul(P.ps[bd][:, :], P.ones_bf[:, :], pT[k2][:, 0, :], start=True, stop=False)
                return e.matmul(P.ps[bd][:, :], P.ones_bf[:, :], pT[k2][:, 1, :], start=False, stop=True)
            sc.op("pe", mm, reads=[MVB, PTB[k2]], writes=[P.psB[bo], P.psB[bd]])
            sc.op("act", lambda e, k2=k2, bd=bd: e.activation(out=rden[k2][:, :], in_=P.ps[bd][:, :], func=AF.Ln), reads=[P.psB[bd]], writes=[RDB[k2]])
            sc.op("act", lambda e, k2=k2: e.activation(out=rden[k2][:, :], in_=rden[k2][:, :], func=AF.Exp, scale=-1.0), reads=[RDB[k2]], writes=[RDB[k2]])
            sc.op("dve", lambda e, k2=k2, bo=bo, hh=hh, s=s: e.tensor_tensor(out=xo[s][:, hh, :], in0=P.ps[bo][:, :], in1=rden[k2][:, :], op=ALU.mult),
                  reads=[P.psB[bo], RDB[k2]], writes=[XOB[s][hh]])
        sc.dma("sp", [(cat_r[:, 8:12, t * NT:(t + 1) * NT], xo[s][:, :, :])], reads=XOB[s], writes=[CatXoB[t]], key=f"xost{s}")
    sc.barrier()
    P.arena_off = mark
    [pool_mix, diff_mix, mlstm_mix, gla_mix][kind](P, li, hall, HallB, w_in_d, cat_r, CatMixB)
    sc.barrier()
    P.arena_off = mark
    wout = P.alloc("wout", [128, 12, D_], BF16)
    WOB = load_w(P, wout, w_out_d, "wout", 3, axis=1)
    Xs = [P.alloc("X", [128, NCH, NT], F32) for _ in range(2)]
    cats = [P.alloc("cat", [128, 12, NT], BF16) for _ in range(2)]
    XB = [bl("X", NCH) for _ in range(2)]
    CB = bl("cat", 2)

    def load3(t):
        s = t % 2
        sc.dma("sp", [(Xs[s][:, :, :], xin_r[:, :, t * NT:(t + 1) * NT])], reads=[XinB[t]], writes=XB[s], key=f"xld{s}")
        sc.dma("sp", [(cats[s][:, :, :], cat_r[:, :, t * NT:(t + 1) * NT])], reads=[CatMixB[t], CatXoB[t]], writes=[CB[s]], key=f"cld{s}")
    load3(0)
    for t in range(NTL):
        s = t % 2
        if t + 1 < NTL:
            load3(t + 1)
        for c in range(NCH):
            bk = P.bank()

            def mm(e, c=c, bk=bk, s=s):
                r = None
                for k in range(12):
                    r = e.matmul(P.ps[bk][:, :], wout[:, k, c * 128:(c + 1) * 128], cats[s][:, k, :], start=(k == 0), stop=(k == 11))
                return r
            sc.op("pe", mm, reads=[CB[s]] + WOB, writes=[P.psB[bk]])
            sc.op("dve", lambda e, c=c, bk=bk, s=s: e.tensor_tensor(out=Xs[s][:, c, :], in0=P.ps[bk][:, :], in1=Xs[s][:, c, :], op=ALU.add),
                  reads=[P.psB[bk], XB[s][c]], writes=[XB[s][c]])
        sc.dma("sp", [(xout_r[:, :, t * NT:(t + 1) * NT], Xs[s][:, :, :])], reads=XB[s], writes=[XoutB[t]], key=f"xst{s}")


def pool_mix(P, li, hall, HallB, w_in_d, cat_r, CatMixB):
    sc = P.sc
    gt = P.vtab
    win = P.alloc("win", [128, NCH, 1024], BF16)
    wg = P.alloc("wg", [128, 8, 256], BF16)
    WINB = load_w(P, win, w_in_d[:, :, 0:1024], "win", 2)
    wg_d = P.dram["pool_w_group"].ap()[0].rearrange("g (i p) o -> p (g i) o", p=128)
    WGB = load_w(P, wg, wg_d, "wg", 1)
    inv0 = P.alloc("inv0", [128, 4, NT], F32)
    INVB = Buf("inv0")
    sc.dma("sp", [(inv0[:, :, :], P.dram["inv0"].ap())], writes=[INVB], key="cst")
    uext = [P.alloc("uext", [128, NCH, 16 + NT], F32) for _ in range(2)]
    wa = P.alloc("wa", [128, 16 + NT], F32)
    wb = P.alloc("wb", [128, 16 + NT], F32)
    pooled = P.alloc("pooled", [128, NCH, NT], BF16)
    mix = [P.alloc("mix", [128, NCH, NT], BF16) for _ in range(2)]
    UB = [bl("u", NCH) for _ in range(2)]
    WAB, WBB = Buf("wa"), Buf("wb")
    PB = bl("pooled", NCH)
    MB = [bl("mix", NCH) for _ in range(2)]
    W2 = NT + 16
    for t in range(NTL):
        s = t % 2
        hv = hall[:, :, t * NT:(t + 1) * NT]
        for c in range(NCH):
            bk = P.bank()
            if t == 0:
                sc.op("dve", lambda e, c=c: e.memset(uext[0][:, c, 0:16], 0.0), writes=[UB[0][c]])
            else:
                sc.op("dve", lambda e, c=c, s=s: e.tensor_copy(out=uext[s][:, c, 0:16], in_=uext[1 - s][:, c, NT:NT + 16]),
                      reads=[UB[1 - s][c]], writes=[UB[s][c]])

            def mm(e, c=c, bk=bk, hv=hv):
                r = None
                for k in range(NCH):
                    r = e.matmul(P.ps[bk][:, :], win[:, k, c * 128:(c + 1) * 128], hv[:, k, :], start=(k == 0), stop=(k == NCH - 1))
                return r
            sc.op("pe", mm, reads=HallB[t] + WINB, writes=[P.psB[bk]])
            sc.op("act", lambda e, c=c, bk=bk, s=s: e.activation(out=uext[s][:, c, 16:W2], in_=P.ps[bk][:, :], func=AF.Copy),
                  reads=[P.psB[bk]], writes=[UB[s][c]])
            g = c // 2
            w = 2 ** (g + 1)
            u = uext[s]
            src_ap = lambda lo, hi, c=c, u=u: u[:, c, lo:hi]
            cur = None
            bufs = [(wa, WAB), (wb, WBB)]
            for i in range(g + 1):
                sh = 2 ** i
                lo = 2 ** (i + 1) - 1
                dst, DB = bufs[i % 2]
                if i == 0:
                    sc.op("dve", lambda e, dst=dst, lo=lo, sh=sh, c=c, u=u: e.tensor_tensor(out=dst[:, lo:W2], in0=u[:, c, lo:W2], in1=u[:, c, lo - sh:W2 - sh], op=ALU.add),
                          reads=[UB[s][c]], writes=[DB])
                else:
                    srcb, SB = bufs[(i - 1) % 2]
                    sc.op("dve", lambda e, dst=dst, srcb=srcb, lo=lo, sh=sh: e.tensor_tensor(out=dst[:, lo:W2], in0=srcb[:, lo:W2], in1=srcb[:, lo - sh:W2 - sh], op=ALU.add),
                          reads=[SB], writes=[DB])
                cur = (dst, DB)
            dst, DB = cur
            if t == 0:
                sc.op("dve", lambda e, dst=dst, g=g: e.tensor_tensor(out=dst[:, 16:W2], in0=dst[:, 16:W2], in1=inv0[:, g, :], op=ALU.mult),
                      reads=[DB, INVB], writes=[DB])
                sc.op("dve", lambda e, dst=dst, c=c, u=u: e.tensor_tensor(out=pooled[:, c, :], in0=dst[:, 16:W2], in1=u[:, c, 16:W2], op=ALU.subtract),
                      reads=[DB, UB[s][c]], writes=[PB[c]])
            else:
                sc.op("dve", lambda e, dst=dst, c=c, u=u, w=w: e.scalar_tensor_tensor(out=pooled[:, c, :], in0=dst[:, 16:W2], scalar=1.0 / w, in1=u[:, c, 16:W2],
                                                                                   op0=ALU.mult, op1=ALU.subtract),
                      reads=[DB, UB[s][c]], writes=[PB[c]])
        for co in range(NCH):
            g, o = co // 2, co % 2
            bk = P.bank()

            def mm(e, g=g, o=o, bk=bk):
                e.matmul(P.ps[bk][:, :], wg[:, g * 2, o * 128:(o + 1) * 128], pooled[:, g * 2, :], start=True, stop=False)
                return e.matmul(P.ps[bk][:, :], wg[:, g * 2 + 1, o * 128:(o + 1) * 128], pooled[:, g * 2 + 1, :], start=False, stop=True)
            sc.op("pe", mm, reads=[PB[g * 2], PB[g * 2 + 1]] + WGB, writes=[P.psB[bk]])
            col = VT["pool_scale"] + co
            sc.op("act", lambda e, co=co, bk=bk, s=s, col=col: e.activation(out=mix[s][:, co, :], in_=P.ps[bk][:, :], func=AF.Copy, scale=gt[:, col:col + 1]),
                  reads=[P.psB[bk], P.ConstB], writes=[MB[s][co]])
        sc.dma("sp", [(cat_r[:, 0:8, t * NT:(t + 1) * NT], mix[s][:, :, :])], reads=MB[s], writes=[CatMixB[t]], key=f"mixst{s}")


def diff_mix(P, li, hall, HallB, w_in_d, cat_r, CatMixB):
    import math
    sc = P.sc
    gt = P.vtab
    H = 8
    lam_init = 0.8 - 0.6 * math.exp(-0.3 * li)
    win = P.alloc("win", [128, NCH, 3072], BF16)
    WINB = load_w(P, win, w_in_d[:, :, 0:3072], "win", 6)
    allW = list(WINB)
    maskd = P.alloc("maskd", [128, 4, NT], BF16)
    ident = P.alloc("ident", [128, 128], BF16)
    CMB = Buf("dconst")
    sc.dma("pool", [(maskd[:, :, :], P.dram["maskd"].ap()), (ident[:, :], P.dram["ident"].ap())], writes=[CMB], key="cstp")
    qa = P.alloc("qa", [128, 2, S_], BF16)
    ka = P.alloc("ka", [128, 2, S_], BF16)
    v = P.alloc("v", [128, 32, 128], BF16)
    QB, KB, VB = bl("qa", NTL), bl("ka", NTL), bl("v", NTL)
    QRB, KRB = Buf("qrows"), Buf("krows")
    pT = [P.alloc("pT", [128, NT], BF16) for _ in range(4)]
    PTB = bl("pT", 4)
    rd = [P.alloc("rd", [128, NT], F32) for _ in range(2)]
    tt = [P.alloc("tt", [128, NT], F32) for _ in range(2)]
    osb = P.alloc("osb", [128, NT], F32)
    sqo = P.alloc("sqo", [128, NT], BF16)
    lnr = P.alloc("lnr", [128, 2, NT], F32)
    mixh = [P.alloc("mixh", [128, NT], BF16) for _ in range(2)]
    RDB, TTB, OSB, SQOB, LNB, MXB = bl("rd", 2), bl("tt", 2), Buf("osb"), Buf("sqo"), bl("lnr", 2), bl("mixh", 2)
    sm = P.alloc("sm", [128, 8], F32)
    pr = P.alloc("pr", [128, 2, 64], F32)
    SMB, PRB = Buf("sm"), Buf("pr")
    lo = VT["diff_lambda"]
    sc.op("dve", lambda e: e.tensor_tensor(out=pr[:, 0, :], in0=gt[:, lo:lo + 64], in1=gt[:, lo + 64:lo + 128], op=ALU.mult), reads=[P.ConstB], writes=[PRB])
    sc.op("dve", lambda e: e.tensor_tensor(out=pr[:, 1, :], in0=gt[:, lo + 128:lo + 192], in1=gt[:, lo + 192:lo + 256], op=ALU.mult), reads=[P.ConstB], writes=[PRB])
    sc.op("dve", lambda e: e.tensor_reduce(out=sm[:, 0:2], in_=pr[:, :, :], axis=AX.X, op=ALU.add), reads=[PRB], writes=[SMB])
    sc.op("act", lambda e: e.activation(out=sm[:, 2:4], in_=sm[:, 0:2], func=AF.Exp), reads=[SMB], writes=[SMB])
    sc.op("dve", lambda e: e.tensor_tensor(out=sm[:, 4:5], in0=sm[:, 2:3], in1=sm[:, 3:4], op=ALU.subtract), reads=[SMB], writes=[SMB])
    sc.op("dve", lambda e: e.tensor_scalar(out=sm[:, 5:6], in0=sm[:, 4:5], scalar1=lam_init, scalar2=-1.0, op0=ALU.add, op1=ALU.mult), reads=[SMB], writes=[SMB])
    dn = VT["diff_norm"]
    sc.op("dve", lambda e: e.tensor_scalar(out=sm[:, 6:7], in0=gt[:, dn:dn + 1], scalar1=1.0 - lam_init, scalar2=None, op0=ALU.mult), reads=[SMB, P.ConstB], writes=[SMB])
    alq_d, alk_d = P.dram["alq"].ap(), P.dram["alk"].ap()
    for h in range(DBG.get('heads', H)):
        sc.dma("pool", [(qa[64:68, 0, :], alq_d[h]), (qa[64:68, 1, :], alq_d[h])], writes=[QRB], key=P.next_wkey())
        sc.dma("pool", [(ka[64:68, 0, :], alk_d[h]), (ka[64:68, 1, :], alk_d[h])], writes=[KRB], key=P.next_wkey())
        for t in range(NTL):
            hv = hall[:, :, t * NT:(t + 1) * NT]
            for isk in range(2):
                for c in range(2):
                    bk = 4 + P.bank() % 4
                    col = isk * 1024 + h * 128 + c * 64

                    def mm(e, bk=bk, col=col, hv=hv):
                        r = None
                        for k in range(NCH):
                            r = e.matmul(P.ps[bk][0:64, :], win[:, k, col:col + 64], hv[:, k, :], start=(k == 0), stop=(k == NCH - 1))
                        return r
                    sc.op("pe", mm, reads=HallB[t] + allW, writes=[P.psB[bk]])
                    if isk == 0:
                        sc.op("act", lambda e, bk=bk, c=c, t=t: e.mul(out=qa[0:64, c, t * NT:(t + 1) * NT], in_=P.ps[bk][0:64, :], mul=0.125),
                              reads=[P.psB[bk]], writes=[QB[t]])
                    else:
                        sc.op("dve", lambda e, bk=bk, c=c, t=t: e.tensor_copy(out=ka[0:64, c, t * NT:(t + 1) * NT], in_=P.ps[bk][0:64, :]),
                              reads=[P.psB[bk]], writes=[KB[t]])
            bk = 4 + P.bank() % 4

            def mmv(e, bk=bk, t=t, h=h):
                r = None
                for tb in range(4):
                    for k in range(NCH):
                        r = e.matmul(P.ps[bk][:, tb * 128:(tb + 1) * 128], hall[:, k, t * NT + tb * 128:t * NT + (tb + 1) * 128],
                                     win[:, k, 2048 + h * 128:2048 + (h + 1) * 128], start=(k == 0), stop=(k == NCH - 1))
                return r
            sc.op("pe", mmv, reads=HallB[t] + allW, writes=[P.psB[bk]])
            sc.op("act", lambda e, bk=bk, t=t: e.activation(out=v[:, 4 * t:4 * t + 4, :], in_=P.ps[bk][:, :].rearrange("p (a b) -> p a b", a=4), func=AF.Copy),
                  reads=[P.psB[bk]], writes=[VB[t]])
        for qt in range(DBG.get('qtiles', NTL)):
            nk = 4 * (qt + 1)
            pend = None
            pcount = 0
            for kc in range(nk + 1):
                cur = None
                if kc < nk:
                    cur = []
                    for c in range(2):
                        bk = 4 + (pcount % 4)
                        pb = pcount % 4
                        pcount += 1
                        j = kc - 4 * qt

                        def mms(e, bk=bk, c=c, kc=kc, j=j, qt=qt):
                            KR = DBG.get('kr', 68)
                            r = e.matmul(P.ps[bk][:, :], ka[0:KR, c, kc * 128:(kc + 1) * 128], qa[0:KR, c, qt * NT:(qt + 1) * NT], start=True, stop=(j < 0))
                            if j >= 0:
                                r = e.matmul(P.ps[bk][:, :], ident[:, :], maskd[:, j, :], start=False, stop=True)
                            return r
                        sc.op("pe", mms, reads=[KB[kc // 4], KRB, QB[qt], QRB, CMB], writes=[P.psB[bk]])
                        sc.op("act", lambda e, bk=bk, pb=pb: e.activation(out=pT[pb][:, :], in_=P.ps[bk][:, :], func=AF.Exp),
                              reads=[P.psB[bk]], writes=[PTB[pb]])
                        cur.append((c, pb, kc))
                if pend is not None:
                    for (c, pb, kk) in pend:
                        def mmp(e, c=c, pb=pb, kk=kk, nk=nk):
                            e.matmul(P.ps[c][:, :], v[:, kk, :], pT[pb][:, :], start=(kk == 0), stop=(kk == nk - 1))
                            return e.matmul(P.ps[2 + c][:, :], P.ones_bf[:, :], pT[pb][:, :], start=(kk == 0), stop=(kk == nk - 1))
                        sc.op("pe", mmp, reads=[VB[kk // 4], PTB[pb]], writes=[P.psB[c], P.psB[2 + c]])
                pend = cur
            for c in range(2):
                sc.op("act", lambda e, c=c: e.activation(out=rd[c][:, :], in_=P.ps[2 + c][:, :], func=AF.Ln), reads=[P.psB[2 + c]], writes=[RDB[c]])
                sc.op("act", lambda e, c=c: e.activation(out=rd[c][:, :], in_=rd[c][:, :], func=AF.Exp, scale=-1.0), reads=[RDB[c]], writes=[RDB[c]])
                sc.op("dve", lambda e, c=c: e.tensor_tensor(out=tt[c][:, :], in0=P.ps[c][:, :], in1=rd[c][:, :], op=ALU.mult),
                      reads=[P.psB[c], RDB[c]], writes=[TTB[c]])
            sc.op("dve", lambda e: e.scalar_tensor_tensor(out=osb[:, :], in0=tt[1][:, :], scalar=sm[:, 5:6], in1=tt[0][:, :], op0=ALU.mult, op1=ALU.add),
                  reads=[TTB[0], TTB[1], SMB], writes=[OSB])
            sc.op("act", lambda e: e.activation(out=sqo[:, :], in_=osb[:, :], func=AF.Square), reads=[OSB], writes=[SQOB])
            bk = 4 + (pcount % 4)
            pcount += 1
            sc.op("pe", lambda e, bk=bk: e.matmul(P.ps[bk][:, :], P.ones_128[:, :], sqo[:, :], start=True, stop=True), reads=[SQOB], writes=[P.psB[bk]])
            sc.op("act", lambda e, bk=bk: e.activation(out=lnr[:, 0, :], in_=P.ps[bk][:, :], func=AF.Ln, bias=1e-5, scale=1.0), reads=[P.psB[bk]], writes=[LNB[0]])
            sc.op("act", lambda e: e.activation(out=lnr[:, 1, :], in_=lnr[:, 0, :], func=AF.Exp, scale=-0.5), reads=[LNB[0]], writes=[LNB[1]])
            s2 = (h * NTL + qt) % 2
            sc.op("dve", lambda e, s2=s2: e.scalar_tensor_tensor(out=mixh[s2][:, :], in0=osb[:, :], scalar=sm[:, 6:7], in1=lnr[:, 1, :], op0=ALU.mult, op1=ALU.mult),
                  reads=[OSB, LNB[1], SMB], writes=[MXB[s2]])
            sc.dma("sp", [(cat_r[:, h, qt * NT:(qt + 1) * NT], mixh[s2][:, :])], reads=[MXB[s2]], writes=[CatMixB[qt]], key=f"mixst{s2}")


def mlstm_mix(P, li, hall, HallB, w_in_d, cat_r, CatMixB):
    lin_mix(P, li, hall, HallB, w_in_d, cat_r, CatMixB, True)


def gla_mix(P, li, hall, HallB, w_in_d, cat_r, CatMixB):
    lin_mix(P, li, hall, HallB, w_in_d, cat_r, CatMixB, False)


def lin_mix(P, li, hall, HallB, w_in_d, cat_r, CatMixB, ML):
    sc = P.sc
    gt = P.vtab
    H = 4
    DKC = 2 if ML else 1
    DVA = 3 if ML else 2
    DV = 256
    DK = 128 * DKC
    NB = S_ // 128
    qscale = float(DK) ** -0.5
    if ML:
        qoff, koff, voff, goff = 0, 1024, 2048, 3072
        ncols = 1024
    else:
        qoff, koff, voff, goff = 0, 512, 1024, 2048
        ncols = 768
    ident = P.alloc("ident", [128, 128], BF16)
    segm = P.alloc("segm", [128, NT], F32)
    cmask = P.alloc("cmask", [128, 64], F32)
    CMB, CM2 = Buf("lconst"), Buf("lconst2")
    sc.dma("pool", [(ident[:, :], P.dram["ident"].ap())], writes=[CMB], key="cstp")
    sc.dma("sp", [(segm[:, :], P.dram["segm"].ap()), (cmask[:, :], P.dram["cmask"].ap())], writes=[CM2], key="cst")
    negb = P.alloc("negb", [128, 8], F32)
    NGB = Buf("negb")
    gb0 = VT["mlstm_gate_b"] if ML else VT["gla_gate_b"]
    sc.op("dve", lambda e: e.tensor_scalar(out=negb[:, 0:8], in0=gt[:, gb0:gb0 + 8], scalar1=-1.0, scalar2=None, op0=ALU.mult),
          reads=[P.ConstB], writes=[NGB])
    win = P.alloc("win", [128, NCH, ncols], BF16)
    if ML:
        wgs = P.alloc("wgs", [128, NCH, 8], BF16)
        wrep = P.alloc("wrep", [128, NCH, 2, 128], BF16)
        WGSB, WREPB = Buf("wgs"), Buf("wrep")
        sc.dma("pool", [(wgs[:, :, :], w_in_d[:, :, 4096:4104])], writes=[WGSB], key=P.next_wkey())
    else:
        wz = P.alloc("wz", [128, NCH, 16], BF16)
        w2 = P.alloc("w2", [16, 512], BF16)
        WZB = Buf("wz")
        sc.dma("pool", [(wz[:, :, :], w_in_d[:, :, 3072:3088]), (w2[:, :], P.dram["gla_gate_w2"].ap()[0])], writes=[WZB], key=P.next_wkey())
    qsL = [P.alloc("qs", [128, DKC, NT], BF16) for _ in range(2)]
    ksL = [P.alloc("ks", [128, DKC, NT], BF16) for _ in range(2)]
    kd = P.alloc("kd", [128, DKC, NT], BF16)
    vtmL = [P.alloc("vtm", [128, 4, DVA * 128], BF16) for _ in range(2)]
    kdtmL = [P.alloc("kdtm", [128, 4, DK], BF16) for _ in range(2)]
    ebL = P.alloc("ebL", [128, 64], F32)
    QSB, KSB, VTB, KDTB, EBLB = bl("qs", 2), bl("ks", 2), bl("vtm", 2), bl("kdtm", 2), bl("ebL", NTL)
    KDB = Buf("kd")
    if ML:
        for s_ in range(2):
            sc.op("pool", lambda e, s_=s_: e.memset(vtmL[s_][:, :, 256:384], 1.0), writes=[VTB[s_]])
        pre = [P.alloc("pre", [128, 4, 3 + NT], F32) for _ in range(2)]
        PREB = [bl("pre", 4) for _ in range(2)]
        cacc = P.alloc("cacc", [128, NT], F32)
        CACB = Buf("cacc")
    qk = P.alloc("qk", [128, 2 * DKC, NT], F32)
    QKB = bl("qk", 2 * DKC)
    g1 = P.alloc("g1", [128, NT], F32)
    g2 = P.alloc("g2", [128, NT], F32)
    g3 = P.alloc("g3", [128, NT], F32)
    g4 = P.alloc("g4", [128, NT], F32)
    g5 = P.alloc("g5", [128, NT], F32)
    g6 = P.alloc("g6", [128, NT], F32)
    G1B, G2B, G3B, G4B, G5B, G6B = (Buf("g1"), Buf("g2"), Buf("g3"), Buf("g4"), Buf("g5"), Buf("g6"))
    zt = P.alloc("zt", [16, NT], BF16)
    ZTB = Buf("zt")
    Sst = P.alloc("Sst", [128, DKC, DVA * 128], F32)
    SbfL = [P.alloc("Sbf", [128, DKC, DVA * 128], BF16) for _ in range(2)]
    SSB = bl("Sst", DKC)
    SBBL = [bl("Sbf", DKC) for _ in range(2)]
    amT = [P.alloc("amT", [128, 128], BF16) for _ in range(2)]
    AMB = bl("amT", 2)
    obuf = [P.alloc("obuf", [128, DVA, NT], F32) for _ in range(2)]
    OBB = [bl("obuf", DVA) for _ in range(2)]
    hn = P.alloc("hn", [128, 2, NT], F32)
    HNB = bl("hn", 2)
    sqn = P.alloc("sqn", [128, 2, NT], BF16)
    SQNB = bl("sqn", 2)
    tmpn = P.alloc("tmpn", [128, 2, NT], F32)
    TNB = bl("tmpn", 2)
    gate = [P.alloc("gate", [128, NT], F32) for _ in range(2)]
    GTB = bl("gate", 2)
    rdn = P.alloc("rdn", [128, NT], F32)
    RDNB = Buf("rdn")
    mixo = [P.alloc("mixo", [128, 2, NT], BF16) for _ in range(2)]
    MXB = [bl("mixo", 2) for _ in range(2)]
    ncol0 = VT["mlstm_norm"] if ML else VT["gla_norm"]
    WB = Buf("winh")
    pbk = [0]

    def pbank():
        pbk[0] = (pbk[0] + 1) % 2
        return 5 + pbk[0]

    for h in range(DBG.get("lheads", H)):
        if ML:
            srcs = [(0, qoff + h * 256, 256), (256, koff + h * 256, 256), (512, voff + h * 256, 256), (768, goff + h * 256, 256)]
        else:
            srcs = [(0, qoff + h * 128, 128), (128, koff + h * 128, 128), (256, voff + h * 256, 256), (512, goff + h * 256, 256)]
        sc.dma("pool", [(win[:, :, d0:d0 + n], w_in_d[:, :, s0:s0 + n]) for (d0, s0, n) in srcs], writes=[WB], key=P.next_wkey())
        wq0, wk0, wv0, wg0 = [s[0] for s in srcs]
        if ML:
            for g in range(2):
                col = g * 4 + h
                sc.op("dve", lambda e, g=g, col=col: e.tensor_copy(out=wrep[:, :, g, :], in_=wgs[:, :, col:col + 1].to_broadcast([128, NCH, 128])),
                      reads=[WGSB], writes=[WREPB])
        for dkc in range(DKC):
            sc.op("dve", lambda e, dkc=dkc: e.memset(Sst[:, dkc, :], 0.0), writes=[SSB[dkc]])
            sc.op("pool", lambda e, dkc=dkc: e.memset(SbfL[0][:, dkc, :], 0.0), writes=[SBBL[0][dkc]])
        def passA(t, h=h):
            hv = hall[:, :, t * NT:(t + 1) * NT]
            HB = HallB[t]
            s = t % 2
            for isk in range(2):
                for dkc in range(DKC):
                    bk = pbank()
                    col = (wk0 if isk else wq0) + dkc * 128
                    idx = isk * DKC + dkc

                    def mm(e, bk=bk, col=col, hv=hv):
                        r = None
                        for k in range(NCH):
                            r = e.matmul(P.ps[bk][:, :], win[:, k, col:col + 128], hv[:, k, :], start=(k == 0), stop=(k == NCH - 1))
                        return r
                    sc.op("pe", mm, reads=HB + [WB], writes=[P.psB[bk]])
                    if ML:
                        pr_ = pre[s]
                        if t == 0:
                            sc.op("dve", lambda e, idx=idx, pr_=pr_: e.memset(pr_[:, idx, 0:3], 0.0), writes=[PREB[s][idx]])
                        else:
                            sc.op("dve", lambda e, idx=idx, pr_=pr_, po=pre[1 - s]: e.tensor_copy(out=pr_[:, idx, 0:3], in_=po[:, idx, NT:NT + 3]),
                                  reads=[PREB[1 - s][idx]], writes=[PREB[s][idx]])
                        sc.op("act", lambda e, idx=idx, pr_=pr_, bk=bk: e.activation(out=pr_[:, idx, 3:3 + NT], in_=P.ps[bk][:, :], func=AF.Copy),
                              reads=[P.psB[bk]], writes=[PREB[s][idx]])
                        c16 = (8 * isk) + 2 * h + dkc
                        cw = VT["conv_w"]
                        sc.op("dve", lambda e, idx=idx, pr_=pr_, c16=c16, cw=cw: e.tensor_scalar(out=cacc[:, :], in0=pr_[:, idx, 3:3 + NT], scalar1=gt[:, cw + 48 + c16:cw + 49 + c16],
                                                                                          scalar2=None, op0=ALU.mult),
                              reads=[PREB[s][idx], P.ConstB], writes=[CACB])
                        for j in range(3):
                            sc.op("dve", lambda e, idx=idx, pr_=pr_, c16=c16, cw=cw, j=j: e.scalar_tensor_tensor(
                                out=cacc[:, :], in0=pr_[:, idx, j:j + NT], scalar=gt[:, cw + j * 16 + c16:cw + j * 16 + c16 + 1], in1=cacc[:, :], op0=ALU.mult, op1=ALU.add),
                                reads=[PREB[s][idx], CACB], writes=[CACB])
                        sc.op("act", lambda e, idx=idx: e.activation(out=qk[:, idx, :], in_=cacc[:, :], func=AF.Silu), reads=[CACB], writes=[QKB[idx]])
                    else:
                        sc.op("act", lambda e, idx=idx, bk=bk: e.activation(out=qk[:, idx, :], in_=P.ps[bk][:, :], func=AF.Copy), reads=[P.psB[bk]], writes=[QKB[idx]])
                    yield
            if ML:
                bi, bf_ = pbank(), pbank()
                for g, bk in ((0, bi), (1, bf_)):
                    def mm(e, g=g, bk=bk, hv=hv):
                        r = None
                        for k in range(NCH):
                            r = e.matmul(P.ps[bk][:, :], wrep[:, k, g, :], hv[:, k, :], start=(k == 0), stop=(k == NCH - 1))
                        return r
                    sc.op("pe", mm, reads=HB + [WREPB], writes=[P.psB[bk]])
                sc.op("act", lambda e, bk=bf_, h=h: e.activation(out=g1[:, :], in_=P.ps[bk][:, :], func=AF.Exp, scale=-1.0, bias=negb[:, 4 + h:5 + h]),
                      reads=[P.psB[bf_], NGB], writes=[G1B])
            else:
                bz = pbank()

                def mm(e, bk=bz, hv=hv):
                    r = None
                    for k in range(NCH):
                        r = e.matmul(P.ps[bk][0:16, :], wz[:, k, :], hv[:, k, :], start=(k == 0), stop=(k == NCH - 1))
                    return r
                sc.op("pe", mm, reads=HB + [WZB], writes=[P.psB[bz]])
                sc.op("act", lambda e, bk=bz: e.activation(out=zt[:, :], in_=P.ps[bk][0:16, :], func=AF.Copy), reads=[P.psB[bz]], writes=[ZTB])
                bx = pbank()
                sc.op("pe", lambda e, bk=bx, h=h: e.matmul(P.ps[bk][:, :], w2[0:16, h * 128:(h + 1) * 128], zt[0:16, :], start=True, stop=True),
                      reads=[ZTB, WZB], writes=[P.psB[bx]])
                sc.op("act", lambda e, bk=bx, h=h: e.activation(out=g1[:, :], in_=P.ps[bk][:, :], func=AF.Exp, scale=-1.0, bias=negb[:, h:h + 1]),
                      reads=[P.psB[bx], NGB], writes=[G1B])
            sc.op("act", lambda e: e.activation(out=g1[:, :], in_=g1[:, :], func=AF.Ln, bias=1.0, scale=1.0), reads=[G1B], writes=[G1B])
            sc.op("dve", lambda e: e.tensor_tensor_scan(out=g2[:, :], data0=segm[:, :], data1=g1[:, :], initial=0.0, op0=ALU.mult, op1=ALU.add),
                  reads=[G1B, CM2], writes=[G2B])
            sc.op("dve", lambda e: e.tensor_scalar(out=g2[:, :], in0=g2[:, :], scalar1=(-1.0 if ML else -1.0 / 16.0), scalar2=None, op0=ALU.mult),
                  reads=[G2B], writes=[G2B])
            sc.op("act", lambda e: e.activation(out=g3[:, :], in_=g2[:, :], func=AF.Exp), reads=[G2B], writes=[G3B])
            if ML:
                sc.op("dve", lambda e, bk=bi, h=h: e.scalar_tensor_tensor(out=g4[:, :], in0=P.ps[bk][:, :], scalar=gt[:, gb0 + h:gb0 + h + 1], in1=g2[:, :],
                                                                   op0=ALU.add, op1=ALU.subtract), reads=[P.psB[bi], G2B, P.ConstB], writes=[G4B])
                sc.op("act", lambda e: e.activation(out=g5[:, :], in_=g4[:, :], func=AF.Exp), reads=[G4B], writes=[G5B])
                for ch in range(8):
                    lc = ch * 64 + 63
                    sc.op("act", lambda e, ch=ch, lc=lc: e.activation(out=g6[:, ch * 64:(ch + 1) * 64], in_=g4[:, ch * 64:(ch + 1) * 64], func=AF.Exp,
                                                                    bias=g2[:, lc:lc + 1], scale=1.0), reads=[G4B, G2B], writes=[G6B])
            else:
                sc.op("act", lambda e: e.activation(out=g5[:, :], in_=g2[:, :], func=AF.Exp, scale=-1.0), reads=[G2B], writes=[G5B])
                for ch in range(8):
                    lc = ch * 64 + 63
                    sc.op("act", lambda e, ch=ch, lc=lc: e.activation(out=g6[:, ch * 64:(ch + 1) * 64], in_=g2[:, ch * 64:(ch + 1) * 64], func=AF.Exp,
                                                                    bias=g2[:, lc:lc + 1], scale=-1.0), reads=[G2B], writes=[G6B])
            yield
            sc.op("act", lambda e, t=t: e.activation(out=ebL[:, t * 8:(t + 1) * 8], in_=g2[:, :].rearrange("p (c l) -> p c l", l=64)[:, :, 63], func=AF.Exp),
                  reads=[G2B], writes=[EBLB[t]])
            yield
            for dkc in range(DKC):
                sc.op("dve", lambda e, dkc=dkc, t=t: e.scalar_tensor_tensor(out=qsL[t % 2][:, dkc, :], in0=qk[:, dkc, :], scalar=qscale, in1=g3[:, :],
                                                                         op0=ALU.mult, op1=ALU.mult), reads=[QKB[dkc], G3B], writes=[QSB[t % 2]])
                sc.op("dve", lambda e, dkc=dkc, t=t: e.tensor_tensor(out=ksL[t % 2][:, dkc, :], in0=qk[:, DKC + dkc, :], in1=g5[:, :], op=ALU.mult),
                      reads=[QKB[DKC + dkc], G5B], writes=[KSB[t % 2]])
                sc.op("dve", lambda e, dkc=dkc: e.tensor_tensor(out=kd[:, dkc, :], in0=qk[:, DKC + dkc, :], in1=g6[:, :], op=ALU.mult),
                      reads=[QKB[DKC + dkc], G6B], writes=[KDB])
            yield
            for tb in range(4):
                bk = pbank()

                def mm(e, bk=bk, tb=tb):
                    r = None
                    for dkc in range(DKC):
                        r = e.matmul(P.ps[bk][:, dkc * 128:(dkc + 1) * 128], kd[:, dkc, tb * 128:(tb + 1) * 128], ident[:, :], start=True, stop=True)
                    return r
                sc.op("pe", mm, reads=[KDB, CMB], writes=[P.psB[bk]])
                sc.op("act", lambda e, bk=bk, tb=tb, t=t: e.activation(out=kdtmL[t % 2][:, tb, :], in_=P.ps[bk][:, 0:DK], func=AF.Copy),
                      reads=[P.psB[bk]], writes=[KDTB[t % 2]])
                bk = pbank()

                def mm(e, bk=bk, tb=tb, t=t):
                    r = None
                    for k in range(NCH):
                        r = e.matmul(P.ps[bk][:, 0:DV], hall[:, k, t * NT + tb * 128:t * NT + (tb + 1) * 128], win[:, k, wv0:wv0 + DV], start=(k == 0), stop=(k == NCH - 1))
                    return r
                sc.op("pe", mm, reads=HB + [WB], writes=[P.psB[bk]])
                sc.op("dve", lambda e, bk=bk, tb=tb, t=t: e.tensor_copy(out=vtmL[t % 2][:, tb, 0:DV], in_=P.ps[bk][:, 0:DV]), reads=[P.psB[bk]], writes=[VTB[t % 2]])
                yield
        def passBC(t, h=h):
            for dc in range(4 * t, 4 * t + 4):
                s = t % 2
                qs, ks, vtm, kdtm = qsL[s], ksL[s], vtmL[s], kdtmL[s]
                cols = slice(dc * 128, (dc + 1) * 128)
                a = dc % 2
                po = 3 + (dc % 2)

                def mms(e, dc=dc, ks=ks, qs=qs):
                    r = None
                    for dkc in range(DKC):
                        r = e.matmul(P.ps[2][:, 0:128], ks[:, dkc, (dc % 4) * 128:(dc % 4 + 1) * 128], qs[:, dkc, (dc % 4) * 128:(dc % 4 + 1) * 128], start=(dkc == 0), stop=(dkc == DKC - 1))
                    return r
                sc.op("pe", mms, reads=[KSB[s], QSB[s]], writes=[P.psB[2]])
                for half in range(2):
                    r0 = half * 64
                    sc.op("dve", lambda e, a=a, r0=r0: e.tensor_tensor(out=amT[a][r0:r0 + 64, r0:r0 + 64], in0=P.ps[2][r0:r0 + 64, r0:r0 + 64], in1=cmask[r0:r0 + 64, :], op=ALU.mult),
                          reads=[P.psB[2], CM2], writes=[AMB[a]])
                for half in range(2):
                    r0 = half * 64
                    ch = dc * 2 + half
                    c0 = (dc % 4) * 128 + r0
                    for dkc in range(DKC):
                        sc.op("pe", lambda e, dkc=dkc, r0=r0, dc=dc, kdtm=kdtm, vtm=vtm: e.matmul(P.ps[dkc][:, 0:DVA * 128], kdtm[r0:r0 + 64, dc % 4, dkc * 128:(dkc + 1) * 128], vtm[r0:r0 + 64, dc % 4, :], start=True, stop=True),
                              reads=[KDTB[s], VTB[s]], writes=[P.psB[dkc]])

                    def mmo(e, a=a, r0=r0, dc=dc, c0=c0, po=po, vtm=vtm, qs=qs, Sbf=SbfL[ch % 2]):
                        r = None
                        for dva in range(DVA):
                            e.matmul(P.ps[po][:, dva * 128 + r0:dva * 128 + r0 + 64], vtm[r0:r0 + 64, dc % 4, dva * 128:(dva + 1) * 128], amT[a][r0:r0 + 64, r0:r0 + 64], start=True, stop=False)
                            for dkc in range(DKC):
                                r = e.matmul(P.ps[po][:, dva * 128 + r0:dva * 128 + r0 + 64], Sbf[:, dkc, dva * 128:(dva + 1) * 128], qs[:, dkc, c0:c0 + 64], start=False, stop=(dkc == DKC - 1))
                        return r
                    sc.op("pe", mmo, reads=[VTB[s], AMB[a], QSB[s]] + SBBL[ch % 2], writes=[P.psB[po]])
                    for dkc in range(DKC):
                        ecol = ch
                        sc.op("dve", lambda e, dkc=dkc, ecol=ecol: e.scalar_tensor_tensor(out=Sst[:, dkc, :], in0=Sst[:, dkc, :], scalar=ebL[:, ecol:ecol + 1], in1=P.ps[dkc][:, 0:DVA * 128],
                                                                                     op0=ALU.mult, op1=ALU.add), reads=[SSB[dkc], EBLB[t], P.psB[dkc]], writes=[SSB[dkc]])
                        if ML:
                            sc.op("act", lambda e, dkc=dkc, Sn=SbfL[(ch + 1) % 2]: e.activation(out=Sn[:, dkc, :], in_=Sst[:, dkc, :], func=AF.Copy), reads=[SSB[dkc]], writes=[SBBL[(ch + 1) % 2][dkc]])
                        else:
                            sc.op("dve", lambda e, dkc=dkc, Sn=SbfL[(ch + 1) % 2]: e.tensor_copy(out=Sn[:, dkc, :], in_=Sst[:, dkc, :]), reads=[SSB[dkc]], writes=[SBBL[(ch + 1) % 2][dkc]])
                    yield
                tb = dc % 4
                for dva in range(DVA):
                    sc.op("act", lambda e, dva=dva, tb=tb, s=s, po=po: e.activation(out=obuf[s][:, dva, tb * 128:(tb + 1) * 128], in_=P.ps[po][:, dva * 128:(dva + 1) * 128], func=AF.Copy),
                          reads=[P.psB[po]], writes=[OBB[s][dva]])
                if tb != 3:
                    continue
                ob = obuf[s]
                if ML:
                    sc.op("dve", lambda e, ob=ob: e.scalar_tensor_tensor(out=rdn[:, :], in0=ob[:, 2, :], scalar=-1.0, in1=ob[:, 2, :], op0=ALU.mult, op1=ALU.max), reads=[OBB[s][2]], writes=[RDNB])
                    sc.op("dve", lambda e: e.tensor_scalar(out=rdn[:, :], in0=rdn[:, :], scalar1=1.0, scalar2=None, op0=ALU.max), reads=[RDNB], writes=[RDNB])
                    sc.op("act", lambda e: e.activation(out=rdn[:, :], in_=rdn[:, :], func=AF.Ln), reads=[RDNB], writes=[RDNB])
                    sc.op("act", lambda e: e.activation(out=rdn[:, :], in_=rdn[:, :], func=AF.Exp, scale=-1.0), reads=[RDNB], writes=[RDNB])
                    for dvc in range(2):
                        sc.op("dve", lambda e, ob=ob, dvc=dvc: e.tensor_tensor(out=ob[:, dvc, :], in0=ob[:, dvc, :], in1=rdn[:, :], op=ALU.mult),
                              reads=[OBB[s][dvc], RDNB], writes=[OBB[s][dvc]])
                nb0 = ncol0 + h * 2
                rmsnorm_tile(P, ob, OBB[s], lambda c: gt[:, nb0 + c:nb0 + c + 1], hn, HNB, sqn, SQNB, tmpn, TNB, nch=2, ones=P.ones_256)
                yield
                hv = hall[:, :, t * NT:(t + 1) * NT]
                for dvc in range(2):
                    bk = pbank()
                    col = wg0 + dvc * 128

                    def mm(e, bk=bk, col=col, hv=hv):
                        r = None
                        for k in range(NCH):
                            r = e.matmul(P.ps[bk][:, :], win[:, k, col:col + 128], hv[:, k, :], start=(k == 0), stop=(k == NCH - 1))
                        return r
                    sc.op("pe", mm, reads=HallB[t] + [WB], writes=[P.psB[bk]])
                    sc.op("act", lambda e, bk=bk, dvc=dvc: e.activation(out=gate[dvc][:, :], in_=P.ps[bk][:, :], func=(AF.Sigmoid if ML else AF.Silu)),
                          reads=[P.psB[bk]], writes=[GTB[dvc]])
                    sc.op("dve", lambda e, dvc=dvc, s=s: e.tensor_tensor(out=mixo[s][:, dvc, :], in0=hn[:, dvc, :], in1=gate[dvc][:, :], op=ALU.mult),
                          reads=[HNB[dvc], GTB[dvc]], writes=[MXB[s][dvc]])
                sc.dma("sp", [(cat_r[:, 2 * h:2 * h + 2, t * NT:(t + 1) * NT], mixo[s][:, :, :])], reads=MXB[s], writes=[CatMixB[t]], key=f"mixst{s}")


        for _ in passA(0):
            pass
        for t in range(NTL):
            ga = passA(t + 1) if t + 1 < NTL else iter(())
            for _ in passBC(t):
                for _k in range(DBG.get("astep", 2)):
                    next(ga, None)
            for _ in ga:
                pass


def _bank(self):
    self._bk = (getattr(self, "_bk", -1) + 1) % 7
    return self._bk


Prog.bank = _bank


def build(nstages=99):
    nc = bass.Bass("TRN2", target_bir_lowering=False)
    P = Prog(nc)
    sc = P.sc
    xT = P.din("xT", [D_, S_])
    memT = P.din("memT", [D_, MEMLEN])
    for k, shp in WEIGHT_SHAPES.items():
        P.din(k, shp)
    vtab_d = P.din("vtab", [128, NV])
    inv0_d = P.din("inv0", [128, 4, NT])
    P.din("maskd", [128, 4, NT])
    P.din("ident", [128, 128])
    P.din("alq", [8, 4, S_])
    P.din("alk", [8, 4, S_])
    P.din("segm", [128, NT])
    P.din("cmask", [128, 64])
    outT = P.dout("outT", [D_, S_])
    xr = P.dint("xr", [D_, S_])
    P.dint("catd", [1536, S_], BF16)
    P.vtab = P.const("vtab_s", [128, NV], F32)
    P.gtab = P.vtab
    P.ones_mean = P.const("ones_mean", [128, 128], BF16)
    P.ones_bf = P.const("ones_bf", [128, 128], BF16)
    P.ones_128 = P.const("ones_128", [128, 128], BF16)
    P.ones_256 = P.const("ones_256", [128, 128], BF16)
    P.mem_n = P.const("mem_n", [128, NCH, MEMLEN], BF16)
    P.ConstB = Buf("consts")
    P.MemNB = Buf("mem_n")
    CB = P.ConstB
    sc.dma("sp", [(P.vtab[:, :], vtab_d.ap())], writes=[CB], key="cst")
    OB = Buf("ones")
    sc.op("dve", lambda e: e.memset(P.ones_mean[:, :], 1.0 / 1024.0), writes=[OB])
    sc.op("dve", lambda e: e.memset(P.ones_bf[:, :], 1.0), writes=[OB])
    sc.op("dve", lambda e: e.memset(P.ones_128[:, :], 1.0 / 128.0), writes=[OB])
    sc.op("dve", lambda e: e.memset(P.ones_256[:, :], 1.0 / 256.0), writes=[OB])
    sc.barrier()
    P.epsc = lambda eps: float(eps)
    P.arena_start()
    P.stage_begin()
    mt = P.alloc("memT", [128, NCH, MEMLEN], F32)
    msq = P.alloc("msq", [128, NCH, MEMLEN], BF16)
    mtmp = P.alloc("mtmp", [128, 2, MEMLEN], F32)
    MTB, MSQB, MTMB = bl("mt", NCH), bl("msq", NCH), bl("mtmp", 2)
    sc.dma("sp", [(mt[:, :, :], memT.ap().rearrange("(c p) n -> p c n", p=128))], writes=MTB, key="xld0")
    mg = VT["norm"] + GI_MEM * NCH
    MNB = bl("memn", NCH)
    rmsnorm_tile(P, mt, MTB, lambda c: P.vtab[:, mg + c:mg + c + 1], P.mem_n, MNB, msq, MSQB, mtmp, MTMB, n=MEMLEN)
    sc.op("dve", lambda e: e.memset(mtmp[:, 0, 0:1], 0.0), reads=MNB, writes=[P.MemNB])
    XinB = bl("xTd", NTL)
    XrB = bl("xrd", NTL)
    OutB = bl("outd", NTL)
    stages = []
    for li in range(DEPTH):
        stages.append(("ffn", li, 0))
        stages.append(("mix", li))
        stages.append(("ffn", li, 1))
    cur, curB = xT, XinB
    sel = stages[:nstages]
    if 'only' in DBG:
        sel = [stages[i] for i in DBG['only']]
    for st in sel:
        if st[0] == "ffn":
            ffn_stage(P, st[1], st[2], cur, xr, curB, XrB)
        else:
            mixer_stage(P, st[1], cur, xr, curB, XrB)
        cur, curB = xr, XrB
    final_stage(P, cur, outT, curB, OutB, norm=(nstages >= len(stages)))
    sc.finish(OutB)
    sc.emit()
    return nc


def host_inputs(inputs, b):
    m = {}
    m["xT"] = np.ascontiguousarray(inputs["x"][b].T)
    m["memT"] = np.ascontiguousarray(inputs["mem"][b].T)
    for k in WEIGHT_SHAPES:
        m[k] = np.ascontiguousarray(inputs[k], dtype=np.float32)
    vt = np.zeros((128, NV), np.float32)

    def cols(v):
        return np.asarray(v, np.float32).reshape(-1, 128).T
    g = np.concatenate([inputs["norm_g"].reshape(12, D_), inputs["final_norm_g"].reshape(1, D_),
                        inputs["mem_norm_g"].reshape(1, D_)], axis=0)
    vt[:, VT["norm"]:VT["norm"] + 14 * NCH] = g.reshape(14, NCH, 128).transpose(2, 0, 1).reshape(128, 14 * NCH)
    vt[:, VT["pool_scale"]:VT["pool_scale"] + 8] = cols(inputs["pool_scale"][0])
    vt[:, VT["diff_norm"]:VT["diff_norm"] + 1] = cols(inputs["diff_norm_g"][0])
    vt[:, VT["mlstm_norm"]:VT["mlstm_norm"] + 8] = cols(inputs["mlstm_norm_g"][0])
    vt[:, VT["gla_norm"]:VT["gla_norm"] + 8] = cols(inputs["gla_norm_g"][0])
    cw = inputs["mlstm_conv_w"][0]
    vt[:, VT["conv_w"]:VT["conv_w"] + 64] = cw.reshape(4, 16, 128).transpose(2, 0, 1).reshape(128, 64)
    vt[:, VT["gla_gate_b"]:VT["gla_gate_b"] + 4] = cols(inputs["gla_gate_b"][0])
    vt[:, VT["mlstm_gate_b"]:VT["mlstm_gate_b"] + 8] = np.broadcast_to(inputs["mlstm_gate_b"][0].reshape(1, 8), (128, 8))
    vt[:, VT["diff_lambda"]:VT["diff_lambda"] + 256] = np.broadcast_to(inputs["diff_lambda"][0].reshape(1, 256), (128, 256))
    m["vtab"] = vt
    m.update(CONSTS)
    return m


def _make_consts():
    c = {}
    inv0 = np.zeros((128, 4, NT), np.float32)
    tpos = np.arange(NT)
    for g, w in enumerate((2, 4, 8, 16)):
        inv0[:, g, :] = (1.0 / np.minimum(tpos + 1, w))[None, :]
    c["inv0"] = inv0
    ki = np.arange(128)[:, None]
    qi = np.arange(NT)[None, :]
    c["maskd"] = np.stack([np.where(qi >= 128 * j + ki, 0.0, -30000.0) for j in range(4)], axis=1).astype(np.float32)
    c["ident"] = np.eye(128, dtype=np.float32)
    pos = np.arange(S_)
    alq = np.zeros((8, 4, S_), np.float32)
    alk = np.zeros((8, 4, S_), np.float32)
    for h in range(8):
        s = 2.0 ** (-(h + 1))
        alq[h] = np.stack([-s * 128.0 * (pos // 128), -s * (pos % 128), np.ones(S_), np.ones(S_)])
        alk[h] = np.stack([np.ones(S_), np.ones(S_), s * 128.0 * (pos // 128), s * (pos % 128)])
    c["alq"], c["alk"] = alq, alk
    c["segm"] = np.broadcast_to((np.arange(NT) % 64 != 0).astype(np.float32)[None, :], (128, NT)).copy()
    sidx = (np.arange(128) % 64)[:, None]
    c["cmask"] = (sidx <= np.arange(64)[None, :]).astype(np.float32)
    return c


CONSTS = _make_consts()


def kernel(**inputs):
    inputs = {k: np.asarray(v) for k, v in inputs.items()}
    nc = build()
    B = inputs["x"].shape[0]
    in_maps = [host_inputs(inputs, b) for b in range(B)]
    res = run_bass_kernel_spmd(nc, in_maps, core_ids=list(range(B)))
    out = np.stack([np.ascontiguousarray(res.results[b]["outT"].T) for b in range(B)], axis=0)
    return out.astype(np.float32)
```
